# Optimizing a Trainium2 kernel written in Bass

```python
import jax
import jax.numpy as jnp
from jax import lax
import numpy as np

D_MODEL = 1024
BATCH = 8
SEQ = 4096
DEPTH = 2

GRID_W = 64
CTX_LEN = 256
EPS = 1e-6
N_MOD = 6

HG_HEADS = 8
HG_DK = 128
HG_DV = 128
HG_WIDTH = HG_HEADS * HG_DK
HG_CHUNK = 32

CONV_WIDTH = D_MODEL
CONV_K = 3

N_HEADS = 16
N_KV_HEADS = 4
HEAD_DIM = 64
GQA_GROUP = N_HEADS // N_KV_HEADS
ATTN_WIDTH = N_HEADS * HEAD_DIM
KV_WIDTH = N_KV_HEADS * HEAD_DIM
Q_BLOCK = 128
ROPE_THETA = 10000.0
ATTN_SCALE = HEAD_DIM ** -0.5

N_BRANCHES = 3
IN_SPLITS = (HG_WIDTH,) * 5 + (CONV_WIDTH,) * 3 + (ATTN_WIDTH, KV_WIDTH, KV_WIDTH) + (D_MODEL,) * N_BRANCHES
IN_WIDTH = 5 * HG_WIDTH + 3 * CONV_WIDTH + ATTN_WIDTH + 2 * KV_WIDTH + N_BRANCHES * D_MODEL

N_EXPERTS = 16
EXPERT_FF = 1024
CAPACITY_FACTOR = 2

kernel_name = 'hybrid_diffusion_hgrn2_conv_gqa_ecmoe'


def rmsnorm(x, g):
    xf = x.astype(jnp.float32)
    y = xf * lax.rsqrt(jnp.mean(xf * xf, axis=-1, keepdims=True) + EPS)
    return (y * g).astype(x.dtype)


def modulation(cvec, w, b):
    mod = jax.nn.silu(cvec) @ w + b
    return jnp.split(mod[:, None, :], N_MOD, axis=-1)


def modulate(h, g, shift, scale):
    return rmsnorm(h, g) * (1 + scale) + shift


def project(h, w_in):
    p = h @ w_in
    return jnp.split(p, np.cumsum(IN_SPLITS)[:-1].tolist(), axis=-1)


def to_heads(t, d):
    return t.reshape(t.shape[0], t.shape[1], -1, d)


def axial_rope_tables(n):
    rows = n // GRID_W
    row = jnp.repeat(jnp.arange(rows), GRID_W).astype(jnp.float32)
    col = jnp.tile(jnp.arange(GRID_W), rows).astype(jnp.float32)
    inv = ROPE_THETA ** (-jnp.arange(0, HEAD_DIM // 2, 2, dtype=jnp.float32) / (HEAD_DIM // 2))
    ang = jnp.concatenate([row[:, None] * inv, col[:, None] * inv], axis=-1)
    return jnp.cos(ang), jnp.sin(ang)


def apply_rope(x, cos, sin):
    x1, x2 = x[..., 0::2], x[..., 1::2]
    c, s = cos[None, :, None, :], sin[None, :, None, :]
    return jnp.stack([x1 * c - x2 * s, x1 * s + x2 * c], axis=-1).reshape(x.shape).astype(x.dtype)


def hgrn_chunk_scan(q, log_f, k, v, s0):
    bsz, n, h, _ = q.shape
    dv = v.shape[-1]
    nc = n // HG_CHUNK

    def chunks(t):
        return t.astype(jnp.float32).reshape(bsz, nc, HG_CHUNK, h, t.shape[-1]).transpose(1, 0, 3, 2, 4)

    lower = jnp.tril(jnp.ones((HG_CHUNK, HG_CHUNK), dtype=bool))[:, :, None]

    def step(s, blk):
        qc, lfc, kc, vc = blk
        b = jnp.cumsum(lfc, axis=2)
        decay = jnp.exp(jnp.where(lower, b[:, :, :, None, :] - b[:, :, None, :, :], -jnp.inf))
        a = jnp.einsum('bhtd,bhsd,bhtsd->bhts', qc, kc, decay)
        o = jnp.einsum('bhts,bhse->bhte', a, vc) + jnp.einsum('bhtd,bhde->bhte', qc * jnp.exp(b), s)
        b_last = b[:, :, -1:, :]
        s_new = jnp.exp(b_last)[:, :, 0, :, None] * s + jnp.einsum('bhsd,bhse->bhde', kc * jnp.exp(b_last - b), vc)
        return s_new, o

    s_fin, o = lax.scan(step, s0, (chunks(q), chunks(log_f), chunks(k), chunks(v)))
    return o.transpose(1, 0, 3, 2, 4).reshape(bsz, n, h, dv), s_fin


def hgrn_direction(q, z, v, lb, s0, reverse):
    z = z.astype(jnp.float32)
    lb = lb.reshape(HG_HEADS, HG_DK)
    log_f = jnp.logaddexp(jnp.log(lb), jnp.log1p(-lb) + jax.nn.log_sigmoid(z))
    k = (1.0 - lb) * jax.nn.sigmoid(-z)
    if reverse:
        q, log_f, k, v = (jnp.flip(t, axis=1) for t in (q, log_f, k, v))
    o, s = hgrn_chunk_scan(q, log_f, k, v, s0)
    if reverse:
        o = jnp.flip(o, axis=1)
    return o, s


def hgrn_readout(o, g, norm_g):
    o = rmsnorm(o, norm_g.reshape(HG_HEADS, HG_DV))
    return o.reshape(o.shape[0], o.shape[1], HG_WIDTH).astype(g.dtype) * jax.nn.silu(g)


def depthwise_conv3(u, w):
    return lax.conv_general_dilated(u, w[:, None, :].astype(u.dtype), window_strides=(1,), padding='SAME',
                                    dimension_numbers=('NWC', 'WIO', 'NWC'), feature_group_count=CONV_WIDTH)


def gqa_softmax(qg, k, v):
    s = jnp.einsum('bqkgd,bskd->bkgqs', qg, k).astype(jnp.float32) * ATTN_SCALE
    p = jax.nn.softmax(s, axis=-1)
    return jnp.einsum('bkgqs,bskd->bqkgd', p.astype(v.dtype), v)


def attend_latent(q, k_lat, v_lat, k_ctx, v_ctx):
    bsz, n = q.shape[:2]
    k_all = jnp.concatenate([k_ctx, k_lat], axis=1)
    v_all = jnp.concatenate([v_ctx, v_lat], axis=1)
    qb = q.reshape(bsz, n // Q_BLOCK, Q_BLOCK, N_KV_HEADS, GQA_GROUP, HEAD_DIM).transpose(1, 0, 2, 3, 4, 5)
    o = lax.map(lambda qi: gqa_softmax(qi, k_all, v_all), qb)
    return o.transpose(1, 0, 2, 3, 4, 5).reshape(bsz, n, ATTN_WIDTH)


def attend_ctx(q, k, v):
    bsz, n = q.shape[:2]
    o = gqa_softmax(q.reshape(bsz, n, N_KV_HEADS, GQA_GROUP, HEAD_DIM), k, v)
    return o.reshape(bsz, n, ATTN_WIDTH)


def merge(pr, o_a, o_b, o_c, w_proj_a, w_proj_b, w_proj_c, w_out):
    ga, gb, gc = (jax.nn.sigmoid(t) for t in pr[11:14])
    y = ga * (o_a @ w_proj_a) + gb * (o_b @ w_proj_b) + gc * (o_c @ w_proj_c)
    return y @ w_out


def token_mixer(h_lat, h_ctx, w_in, lb_fwd, lb_bwd, hg_norm_g, conv_w, q_norm_g, k_norm_g,
                w_proj_a, w_proj_b, w_proj_c, w_out, cos, sin, ctx_out):
    pl = project(h_lat, w_in)
    pc = project(h_ctx, w_in)
    bsz = h_lat.shape[0]
    zero_state = jnp.zeros((bsz, HG_HEADS, HG_DK, HG_DV), jnp.float32)
    qc_h, ic_h = to_heads(pc[0], HG_DK), to_heads(pc[3], HG_DV)
    ql_h, il_h = to_heads(pl[0], HG_DK), to_heads(pl[3], HG_DV)
    oc_f, sc_f = hgrn_direction(qc_h, to_heads(pc[1], HG_DK), ic_h, lb_fwd, zero_state, False)
    oc_b, sc_b = hgrn_direction(qc_h, to_heads(pc[2], HG_DK), ic_h, lb_bwd, zero_state, True)
    ol_f, _ = hgrn_direction(ql_h, to_heads(pl[1], HG_DK), il_h, lb_fwd, sc_f, False)
    ol_b, _ = hgrn_direction(ql_h, to_heads(pl[2], HG_DK), il_h, lb_bwd, sc_b, True)
    a_lat = hgrn_readout(ol_f + ol_b, pl[4], hg_norm_g)
    b_lat = pl[5] * depthwise_conv3(pl[6] * pl[7], conv_w)
    q_l = apply_rope(rmsnorm(to_heads(pl[8], HEAD_DIM), q_norm_g), cos, sin)
    k_l = apply_rope(rmsnorm(to_heads(pl[9], HEAD_DIM), k_norm_g), cos, sin)
    v_l = to_heads(pl[10], HEAD_DIM)
    k_c = rmsnorm(to_heads(pc[9], HEAD_DIM), k_norm_g)
    v_c = to_heads(pc[10], HEAD_DIM)
    c_lat = attend_latent(q_l, k_l, v_l, k_c, v_c)
    y_lat = merge(pl, a_lat, b_lat, c_lat, w_proj_a, w_proj_b, w_proj_c, w_out)
    if not ctx_out:
        return y_lat, None
    a_ctx = hgrn_readout(oc_f + oc_b, pc[4], hg_norm_g)
    b_ctx = pc[5] * depthwise_conv3(pc[6] * pc[7], conv_w)
    q_c = rmsnorm(to_heads(pc[8], HEAD_DIM), q_norm_g)
    att_c = attend_ctx(q_c, k_c, v_c)
    y_ctx = merge(pc, a_ctx, b_ctx, att_c, w_proj_a, w_proj_b, w_proj_c, w_out)
    return y_lat, y_ctx


def expert_choice_ffn(h, router_w, w_gate, w_up, w_down):
    bsz, n, d = h.shape
    cap = CAPACITY_FACTOR * n // N_EXPERTS
    aff = jax.nn.softmax((h @ router_w).astype(jnp.float32), axis=-1)
    w_sel, idx = lax.top_k(aff.transpose(0, 2, 1), cap)
    xs = jax.vmap(lambda hb, ib: hb[ib])(h, idx)
    hid = jax.nn.silu(jnp.einsum('becd,edf->becf', xs, w_gate)) * jnp.einsum('becd,edf->becf', xs, w_up)
    ys = jnp.einsum('becf,efd->becd', hid, w_down) * w_sel[..., None].astype(h.dtype)
    return jax.vmap(lambda yb, ib: jnp.zeros((n, d), h.dtype).at[ib.reshape(-1)].add(yb.reshape(-1, d)))(ys, idx)


def setup_inputs(seed: int = 0) -> dict:
    key = jax.random.key(seed)
    ks = jax.random.split(key, 23)

    def nrm(k, shape, scale):
        return jax.random.normal(k, shape, jnp.float32) * scale

    return {
        'x': nrm(ks[0], (BATCH, SEQ, D_MODEL), 1.0),
        'c': nrm(ks[1], (BATCH, D_MODEL), 1.0),
        'ctx': nrm(ks[2], (BATCH, CTX_LEN, D_MODEL), 1.0),
        'c_ctx': nrm(ks[3], (D_MODEL,), 1.0),
        'ada_w': nrm(ks[4], (DEPTH, D_MODEL, N_MOD * D_MODEL), 0.5 * D_MODEL ** -0.5),
        'ada_b': nrm(ks[5], (DEPTH, N_MOD * D_MODEL), 0.02),
        'norm1_g': 1.0 + nrm(ks[6], (DEPTH, D_MODEL), 0.1),
        'norm2_g': 1.0 + nrm(ks[7], (DEPTH, D_MODEL), 0.1),
        'w_in': nrm(ks[8], (DEPTH, D_MODEL, IN_WIDTH), D_MODEL ** -0.5),
        'hg_lb_logits': nrm(ks[9], (DEPTH, 2, HG_WIDTH), 1.0),
        'hg_norm_g': 1.0 + nrm(ks[10], (DEPTH, HG_WIDTH), 0.1),
        'conv_w': nrm(ks[11], (DEPTH, CONV_K, CONV_WIDTH), CONV_K ** -0.5),
        'q_norm_g': 1.0 + nrm(ks[12], (DEPTH, HEAD_DIM), 0.1),
        'k_norm_g': 1.0 + nrm(ks[13], (DEPTH, HEAD_DIM), 0.1),
        'w_proj_a': nrm(ks[14], (DEPTH, HG_WIDTH, D_MODEL), HG_WIDTH ** -0.5),
        'w_proj_b': nrm(ks[15], (DEPTH, CONV_WIDTH, D_MODEL), CONV_WIDTH ** -0.5),
        'w_proj_c': nrm(ks[16], (DEPTH, ATTN_WIDTH, D_MODEL), ATTN_WIDTH ** -0.5),
        'w_out': nrm(ks[17], (DEPTH, D_MODEL, D_MODEL), D_MODEL ** -0.5),
        'router_w': nrm(ks[18], (DEPTH, D_MODEL, N_EXPERTS), D_MODEL ** -0.5),
        'w_gate': nrm(ks[19], (DEPTH, N_EXPERTS, D_MODEL, EXPERT_FF), D_MODEL ** -0.5),
        'w_up': nrm(ks[20], (DEPTH, N_EXPERTS, D_MODEL, EXPERT_FF), D_MODEL ** -0.5),
        'w_down': nrm(ks[21], (DEPTH, N_EXPERTS, EXPERT_FF, D_MODEL), EXPERT_FF ** -0.5),
        'final_norm_g': 1.0 + nrm(ks[22], (D_MODEL,), 0.1),
    }


def reference(x, c, ctx, c_ctx, ada_w, ada_b, norm1_g, norm2_g, w_in, hg_lb_logits, hg_norm_g, conv_w,
              q_norm_g, k_norm_g, w_proj_a, w_proj_b, w_proj_c, w_out, router_w, w_gate, w_up, w_down,
              final_norm_g):
    cos, sin = axial_rope_tables(x.shape[1])
    lb = jnp.cumsum(jax.nn.softmax(hg_lb_logits.astype(jnp.float32), axis=0), axis=0)
    lb = lb - lb[:1]
    h_ctx = ctx
    for l in range(DEPTH):
        ctx_out = l < DEPTH - 1
        sh_a, sc_a, g_a, sh_f, sc_f, g_f = modulation(c, ada_w[l], ada_b[l])
        csh_a, csc_a, cg_a, csh_f, csc_f, cg_f = modulation(c_ctx[None, :], ada_w[l], ada_b[l])
        y_lat, y_ctx = token_mixer(modulate(x, norm1_g[l], sh_a, sc_a), modulate(h_ctx, norm1_g[l], csh_a, csc_a),
                                   w_in[l], lb[l, 0], lb[l, 1], hg_norm_g[l], conv_w[l], q_norm_g[l], k_norm_g[l],
                                   w_proj_a[l], w_proj_b[l], w_proj_c[l], w_out[l], cos, sin, ctx_out)
        x = x + g_a * y_lat
        x = x + g_f * expert_choice_ffn(modulate(x, norm2_g[l], sh_f, sc_f), router_w[l], w_gate[l], w_up[l], w_down[l])
        if ctx_out:
            h_ctx = h_ctx + cg_a * y_ctx
            h_ctx = h_ctx + cg_f * expert_choice_ffn(modulate(h_ctx, norm2_g[l], csh_f, csc_f),
                                                     router_w[l], w_gate[l], w_up[l], w_down[l])
    return rmsnorm(x, final_norm_g)
```

```python
import numpy as np
import concourse.bass as bass
import concourse.mybir as mybir
from concourse.bass_utils import run_bass_kernel_spmd
from contextlib import ExitStack

F32 = mybir.dt.float32
BF16 = mybir.dt.bfloat16
ALU = mybir.AluOpType
AF = mybir.ActivationFunctionType

D = 1024; KC = 8; TC = 256; TL = 4096; T = TC + TL; NT = T // 128
DEPTH = 2; NE = 16; EPS = 1e-6; CH = 32
BLOCKS = [(0, 256)] + [(256 + 512 * i, 512) for i in range(8)]
B256 = [(256 * i, 256) for i in range(17)]
O_HQ, O_HF, O_HB, O_HI, O_HG, O_CB, O_CC, O_CX, O_AQ, O_AK, O_AV, O_GA, O_GB, O_GC = (
    0, 1024, 2048, 3072, 4096, 5120, 6144, 7168, 8192, 9216, 9472, 9728, 10752, 11776)
NDS = 6


class KB:
    def __init__(self, nc, es):
        self.nc = nc
        self.E = {'pe': nc.tensor, 'act': nc.scalar, 'dve': nc.vector, 'pool': nc.gpsimd, 'sp': nc.sync}
        self.esem = {k: es.enter_context(nc.semaphore('e_' + k)) for k in self.E}
        self.ecnt = {k: 0 for k in self.E}
        self.seen = {k: {} for k in self.E}
        self.lastw = {}
        self.readers = {}
        self.dsem = {q: [es.enter_context(nc.semaphore('d_%s%d' % (q, i))) for i in range(NDS)] for q in ('sp', 'pool', 'act')}
        self.dcnt = {q: [0] * NDS for q in self.dsem}
        self.dnext = {q: 0 for q in self.dsem}
        self.ninstr = 0

    def _wait(self, eng, tok):
        key, sem, val = tok
        if self.seen[eng].get(key, 0) >= val:
            return
        self.E[eng].wait_ge(sem, val)
        self.seen[eng][key] = val

    def _deps(self, eng, reads, writes):
        toks = []
        for r in reads:
            t = self.lastw.get(r)
            if t is not None:
                toks.append(t)
        for w in writes:
            t = self.lastw.get(w)
            if t is not None:
                toks.append(t)
            rd = self.readers.get(w)
            if rd:
                toks.extend(rd.values())
        for t in toks:
            if eng == 'pe' and t[0] == 'pe':
                continue
            self._wait(eng, t)

    def _commit(self, tok, reads, writes):
        for r in reads:
            self.readers.setdefault(r, {})[tok[0]] = tok
        for w in writes:
            self.lastw[w] = tok
            self.readers[w] = {}

    def op(self, eng, reads, writes, fn):
        self._deps(eng, reads, writes)
        ins = fn()
        self.ecnt[eng] += 1
        ins.then_inc(self.esem[eng], 1)
        self._commit((eng, self.esem[eng], self.ecnt[eng]), reads, writes)
        self.ninstr += 1

    def dma(self, q, out, in_, reads, writes, **kw):
        self._deps(q, reads, writes)
        i = self.dnext[q]
        self.dnext[q] = (i + 1) % NDS
        sem = self.dsem[q][i]
        key = ('d', q, i)
        if self.dcnt[q][i] > 0:
            self._wait(q, (key, sem, self.dcnt[q][i]))
        self.dcnt[q][i] += 16
        self.E[q].dma_start(out=out, in_=in_, **kw).then_inc(sem, 16)
        self._commit((key, sem, self.dcnt[q][i]), reads, writes)
        self.ninstr += 1

    def barrier(self):
        engs = ['pe', 'act', 'dve', 'pool', 'sp']
        toks = [(e, self.esem[e], self.ecnt[e]) for e in engs if self.ecnt[e]]
        for q in self.dsem:
            for i in range(NDS):
                if self.dcnt[q][i]:
                    toks.append((('d', q, i), self.dsem[q][i], self.dcnt[q][i]))
        for e in engs:
            for t in toks:
                if t[0] == e:
                    continue
                self._wait(e, t)
        self.lastw.clear()
        self.readers.clear()

    def finish(self, keys):
        self.barrier()


class Prog:
    def __init__(self, nlayers=DEPTH, stop=None, dbg=()):
        self.nlayers = nlayers
        self.stop = stop
        self.dbg = dbg
        nc = self.nc = bass.Bass("TRN2", target_bir_lowering=False)
        self.es = ExitStack()
        self.inputs = {}

    def din(self, name, shape, dt=F32):
        t = self.nc.dram_tensor(name, list(shape), dt, kind="ExternalInput").ap()
        self.inputs[name] = t
        return t

    def dout(self, name, shape, dt=F32):
        return self.nc.dram_tensor(name, list(shape), dt, kind="ExternalOutput").ap()

    def dscr(self, name, shape, dt):
        return self.nc.dram_tensor(name, list(shape), dt, kind="Internal").ap()

    def sb(self, name, shape, dt):
        return self.es.enter_context(self.nc.sbuf_tensor(name, list(shape), dt))

    def ps(self, name, shape, dt):
        return self.es.enter_context(self.nc.psum_tensor(name, list(shape), dt))

    def MM(self, out, lhsT, rhs, st, sp, R, W, **kw):
        self.kb.op('pe', R, W, lambda: self.nc.tensor.matmul(out, lhsT, rhs, start=st, stop=sp, **kw))

    def TR(self, out, in_, ident, R, W):
        self.kb.op('pe', R, W, lambda: self.nc.tensor.transpose(out, in_, ident))

    def ACT(self, out, in_, func, R, W, scale=1.0, bias=0.0):
        self.kb.op('act', R, W, lambda: self.nc.scalar.activation(out, in_, func, bias=bias, scale=scale))

    def TT(self, eng, out, in0, in1, op, R, W):
        e = self.nc.vector if eng == 'dve' else self.nc.gpsimd
        self.kb.op(eng, R, W, lambda: e.tensor_tensor(out, in0, in1, op))

    def TS(self, eng, out, in0, s1, op0, R, W, s2=None, op1=None, accum_out=None):
        e = self.nc.vector if eng == 'dve' else self.nc.gpsimd
        if op1 is None:
            self.kb.op(eng, R, W, lambda: e.tensor_scalar(out, in0, s1, s2, op0, accum_out=accum_out))
        else:
            self.kb.op(eng, R, W, lambda: e.tensor_scalar(out, in0, s1, s2, op0, op1, accum_out=accum_out))

    def STT(self, out, in0, scalar, in1, op0, op1, R, W):
        self.kb.op('dve', R, W, lambda: self.nc.vector.scalar_tensor_tensor(out, in0, scalar, in1, op0, op1))

    def CP(self, eng, out, in_, R, W):
        if eng == 'act':
            self.kb.op('act', R, W, lambda: self.nc.scalar.copy(out, in_))
        else:
            e = self.nc.vector if eng == 'dve' else self.nc.gpsimd
            self.kb.op(eng, R, W, lambda: e.tensor_copy(out, in_))

    def RCP(self, out, in_, R, W):
        self.kb.op('dve', R, W, lambda: self.nc.vector.reciprocal(out, in_))

    def MS(self, eng, ap, val, W):
        e = self.nc.vector if eng == 'dve' else self.nc.gpsimd
        self.kb.op(eng, [], W, lambda: e.memset(ap, val))

    def DMA(self, q, out, in_, R, W, **kw):
        self.kb.dma(q, out, in_, R, W, **kw)

    def build(self):
        nc = self.nc
        with self.es:
            self.kb = KB(nc, self.es)
            self.declare()
            self.setup_consts()
            self.phase_load_x()
            done = True
            for l in range(self.nlayers):
                self.layer(l)
                if self.stop is not None and self.stop[0] == l:
                    done = False
                    break
            if done:
                self.phase_final()
            self.debug_dump()
            self.kb.finish([])
        return nc

    def declare(self):
        L = DEPTH
        self.x = self.din("x", [TL, D]); self.ctx = self.din("ctx", [TC, D])
        self.cc = self.din("cc", [128, 16])
        self.ada_w = self.din("ada_w", [L, D, 6 * D]); self.ada_b = self.din("ada_b", [L, 128, 48])
        self.n1g = self.din("n1g", [L, 128, 8]); self.n2g = self.din("n2g", [L, 128, 8]); self.fng = self.din("fng", [128, 8])
        self.hgn = self.din("hgn", [L, 128, 8]); self.convw = self.din("convw", [L, 128, 24])
        self.qng = self.din("qng", [L, 128, 1]); self.kng = self.din("kng", [L, 128, 1])
        self.lbl = self.din("lbl", [128, 32])
        self.w_in = self.din("w_in", [L, D, 12800]); self.wk_dup = self.din("wk_dup", [L, D, 512])
        self.wpa = self.din("w_proj_a", [L, D, D]); self.wpb = self.din("w_proj_b", [L, D, D])
        self.wpc = self.din("w_proj_c", [L, D, D]); self.wout = self.din("w_out", [L, D, D])
        self.rw = self.din("router_w", [L, D, NE])
        self.wg = self.din("w_gate", [L, NE, D, D]); self.wu = self.din("w_up", [L, NE, D, D]); self.wd = self.din("w_down", [L, NE, D, D])
        self.c_ident = self.din("c_ident", [128, 128]); self.c_blk64 = self.din("c_blk64", [128, 128]); self.c_swap = self.din("c_swap", [128, 128])
        self.c_maskF = self.din("c_maskF", [128, 32]); self.c_maskB = self.din("c_maskB", [128, 32]); self.c_rowm = self.din("c_rowm", [128, 4])
        self.c_mask01 = self.din("c_mask01", [128, 512]); self.c_iota = self.din("c_iota", [128, 512]); self.c_jcol = self.din("c_jcol", [128, 4])
        self.c_cos = self.din("c_cos", [128, TL]); self.c_sin = self.din("c_sin", [128, TL])
        self.out = self.dout("out", [TL, D])
        self.xT = self.dscr("xT", [128, KC, T], F32)
        self.oa = self.dscr("s_oa", [128, KC, T], BF16); self.ob = self.dscr("s_ob", [128, KC, T], BF16); self.oc = self.dscr("s_oc", [128, KC, T], BF16)
        self.ga = self.dscr("s_ga", [128, KC, T], BF16); self.gb = self.dscr("s_gb", [128, KC, T], BF16); self.gc = self.dscr("s_gc", [128, KC, T], BF16)
        self.ysl = self.dscr("s_ysl", [NE, 128, 4, D], BF16); self.ysc = self.dscr("s_ysc", [NE, 32, D], BF16)
        self.posm_d = self.dscr("s_posm", [NE, T], F32); self.wm_d = self.dscr("s_wm", [NE, T], F32)
        self.vtok = self.dscr("s_vtok", [T // 64, 64, D], BF16)
        self.ofd = self.dscr("s_ofd", [128, KC, T], F32); self.obd = self.dscr("s_obd", [128, KC, T], F32)

        self.dbg_outs = []

    def scope(self, psum='default'):
        prog = self

        class _S:
            def __enter__(s):
                s.es = ExitStack(); s.es.__enter__()
                prog.uid = getattr(prog, 'uid', 0) + 1
                u = prog.uid
                pt = lambda name, shape, dt: s.es.enter_context(prog.nc.psum_tensor("%s_u%d" % (name, u), shape, dt))
                prog.P01 = pt("P01", [128, 1024], F32); prog.P23 = pt("P23", [128, 1024], F32); prog.P45 = pt("P45", [128, 1024], F32)
                prog.P6 = pt("P6", [128, 512], F32)
                if psum == 'attn':
                    prog.P7 = pt("P7", [128, 512], F32); prog.PT = None
                else:
                    prog.PT = pt("PT", [128, 1024], BF16); prog.P7 = None
                prog.P0 = prog.P01[:, 0:512]; prog.P1 = prog.P01[:, 512:1024]; prog.P2 = prog.P23[:, 0:512]; prog.P3 = prog.P23[:, 512:1024]
                prog.P4 = prog.P45[:, 0:512]; prog.P5 = prog.P45[:, 512:1024]
                return s

            def sb(s, name, shape, dt):
                prog.uid = getattr(prog, 'uid', 0) + 1
                return s.es.enter_context(prog.nc.sbuf_tensor("%s_u%d" % (name, prog.uid), list(shape), dt))

            def __exit__(s, *a):
                if a[0] is None:
                    prog.kb.barrier()
                return s.es.__exit__(*a)
        return _S()

    def setup_consts(self):
        sb = self.sb
        self.ident = sb("ident", [128, 128], F32); self.identb = sb("identb", [128, 128], BF16)
        self.blk64 = sb("blk64", [128, 128], F32); self.swapP = sb("swapP", [128, 128], F32)
        self.onesb = sb("onesb", [128, 512], BF16); self.onesf = sb("onesf", [128, 128], F32); self.zerob = sb("zerob", [128, 128], BF16)
        self.maskF = sb("maskF", [128, 32], F32); self.maskB = sb("maskB", [128, 32], F32); self.rowm = sb("rowm", [128, 4], F32)
        self.mask01 = sb("mask01", [128, 512], F32); self.iota = sb("iota", [128, 512], F32); self.jcol = sb("jcol", [128, 4], F32)
        for nm, dst, src in (('ident', self.ident, self.c_ident), ('blk64', self.blk64, self.c_blk64), ('swapP', self.swapP, self.c_swap),
                             ('maskF', self.maskF, self.c_maskF), ('maskB', self.maskB, self.c_maskB), ('rowm', self.rowm, self.c_rowm), ('mask01', self.mask01, self.c_mask01),
                             ('iota', self.iota, self.c_iota), ('jcol', self.jcol, self.c_jcol)):
            self.DMA('sp', dst[:], src, [], [nm])
        self.CP('dve', self.identb[:], self.ident[:], ['ident'], ['identb'])
        self.MS('dve', self.onesb[:], 1.0, ['onesb']); self.MS('dve', self.onesf[:], 1.0, ['onesf']); self.MS('dve', self.zerob[:], 0.0, ['zerob'])
        self.scc = sb("scc", [128, 16], F32)
        self.DMA('sp', self.scc[:], self.cc, [], ['scc'])
        self.ACT(self.scc[:], self.scc[:], AF.Silu, ['scc'], ['scc'])
        self.lb = sb("lb", [128, 32], F32)
        self.DMA('sp', self.lb[:], self.lbl, [], ['lb'])
        self.TT('dve', self.lb[:, 16:32], self.lb[:, 16:32], self.lb[:, 0:16], ALU.subtract, ['lb'], ['lb'])
        self.ACT(self.lb[:, 16:32], self.lb[:, 16:32], AF.Sigmoid, ['lb'], ['lb'])
        self.MS('dve', self.lb[:, 0:16], 0.0, ['lb'])
        self.oml = sb("oml", [128, 32], F32)
        self.ACT(self.oml[:], self.lb[:], AF.Identity, ['lb'], ['oml'], scale=-1.0, bias=1.0)
        self.fngs = sb("fngs", [128, 8], F32)
        self.DMA('sp', self.fngs[:], self.fng, [], ['fngs'])
        self.modv = sb("modv", [128, 48, 2], F32); self.G1 = sb("G1", [128, 8, 2], F32); self.G2 = sb("G2", [128, 8, 2], F32)
        self.vecs = sb("vecs", [128, 64], F32); self.vec2 = sb("vec2", [128, 64], F32)
        self.ptok = sb("ptok", [128, NT, NE], F32)
        self.kb.barrier()

    def wload(self, dst, src2d, key, c0=0, c1=None):
        v = src2d.rearrange("(k p) n -> p k n", p=128)
        if c1 is not None:
            v = v[:, :, c0:c1]
        n = v.shape[2]
        for k0 in range(0, 8, 2):
            self.DMA('pool', dst[:, k0:k0 + 2, :n], v[:, k0:k0 + 2, :], [], [key])

    def phase_load_x(self):
        with self.scope() as S:
            xbs = [S.sb("lx_xb%d" % i, [128, KC, 512], F32) for i in range(2)]
            stg = [S.sb("lx_st%d" % i, [128, 512], F32) for i in range(4)]
            for bi, (t0, W) in enumerate(BLOCKS):
                xb = xbs[bi % 2]; kx = 'xb%d' % (bi % 2)
                for j in range(W // 128):
                    tok = t0 + j * 128
                    src = self.ctx[tok:tok + 128, :] if tok < TC else self.x[tok - TC:tok - TC + 128, :]
                    for hf in range(2):
                        si = (j % 2) * 2 + hf
                        st = stg[si]; ks = 'st%d' % si
                        self.DMA('sp', st[:], src[:, hf * 512:(hf + 1) * 512], [], [ks])
                        pb = self.P0 if hf == 0 else self.P1; kp = 'P0' if hf == 0 else 'P1'
                        for c4 in range(4):
                            self.TR(pb[:, c4 * 128:(c4 + 1) * 128], st[:, c4 * 128:(c4 + 1) * 128], self.ident[:], [ks, 'ident'], [kp])
                        self.CP('act' if hf else 'dve', xb[:, hf * 4:(hf + 1) * 4, j * 128:(j + 1) * 128],
                                pb.rearrange("p (c t) -> p c t", c=4), [kp], [kx])
                self.DMA('sp', self.xT[:, :, t0:t0 + W], xb[:, :, :W], [kx], [('xT', bi)])

    def layer(self, l):
        last = (l == DEPTH - 1)
        self.phase_mod(l)
        hs = ExitStack(); hs.__enter__()
        self.hT = hs.enter_context(self.nc.sbuf_tensor("hT%d" % l, [128, KC, T], BF16))
        self.phase_norm1(l)
        if self.stop == (l, 'n1'):
            self.debug_dump(); self.kb.barrier(); hs.close(); return
        self.phase_attn(l)
        if self.stop == (l, 'attn'):
            self.kb.barrier(); hs.close(); return
        self.phase_conv(l)
        if self.stop == (l, 'conv'):
            self.kb.barrier(); hs.close(); return
        self.phase_hgrn(l)
        if self.stop == (l, 'hgrn'):
            self.kb.barrier(); hs.close(); return
        self.phase_gates(l)
        self.kb.barrier(); hs.close()
        self.phase_merge(l)
        if self.stop == (l, 'merge'):
            return
        hs = ExitStack(); hs.__enter__()
        self.h2t = hs.enter_context(self.nc.sbuf_tensor("h2t%d" % l, [128, NT, D], BF16))
        self.phase_router(l)
        self.phase_moeA(l)
        self.kb.barrier(); hs.close()
        self.phase_moeB(l)

    def groups(self, l):
        g = [dict(name='l', tok0=TC, ntok=TL, cap=512, j=0)]
        if l < DEPTH - 1:
            g.append(dict(name='c', tok0=0, ntok=TC, cap=32, j=1))
        return g

    def phase_mod(self, l):
        vs = self.vecs
        with self.scope() as S:
            wts = [S.sb("adaw%d" % i, [128, 8, 768], F32) for i in range(2)]
            self.DMA('sp', vs[:, 0:48], self.ada_b[l], [], ['vecs'])
            self.DMA('sp', vs[:, 48:56], self.n1g[l], [], ['vecs'])
            self.DMA('sp', vs[:, 56:64], self.n2g[l], [], ['vecs'])
            self.DMA('sp', self.vec2[:, 0:8], self.hgn[l], [], ['vec2'])
            self.DMA('sp', self.vec2[:, 8:32], self.convw[l], [], ['vec2'])
            self.DMA('sp', self.vec2[:, 32:33], self.qng[l], [], ['vec2'])
            self.DMA('sp', self.vec2[:, 33:34], self.kng[l], [], ['vec2'])
            for grp in range(8):
                wt = wts[grp % 2]; kw = 'adaw%d' % (grp % 2)
                self.DMA('sp', wt[:], self.ada_w[l].rearrange("(k p) n -> p k n", p=128)[:, :, grp * 768:(grp + 1) * 768], [], [kw])
                for m in range(6):
                    mc = grp * 6 + m
                    for kc in range(8):
                        self.MM(self.P4[:, mc * 2:mc * 2 + 2], wt[:, kc, m * 128:(m + 1) * 128], self.scc[:, kc::8], kc == 0, kc == 7, [kw, 'scc'], ['P4'])
            pv = self.P4[:, 0:96].rearrange("p (m j) -> p m j", j=2)
            for j in range(2):
                self.TT('dve', self.modv[:, :, j], pv[:, :, j], vs[:, 0:48], ALU.add, ['P4', 'vecs'], ['modv'])
            for j in range(2):
                self.TS('dve', self.G1[:, :, j], self.modv[:, 8:16, j], 1.0, ALU.add, ['modv'], ['G1'])
                self.TT('dve', self.G1[:, :, j], self.G1[:, :, j], vs[:, 48:56], ALU.mult, ['G1', 'vecs'], ['G1'])
                self.TS('dve', self.G2[:, :, j], self.modv[:, 32:40, j], 1.0, ALU.add, ['modv'], ['G2'])
                self.TT('dve', self.G2[:, :, j], self.G2[:, :, j], vs[:, 56:64], ALU.mult, ['G2', 'vecs'], ['G2'])

    def rstd_from_ps(self, ps, kps, rs, krs, W, nfeat):
        self.ACT(rs, ps, AF.Sqrt, [kps], [krs], scale=1.0 / nfeat, bias=EPS)
        self.RCP(rs, rs, [krs], [krs])

    def norm_block(self, S_, xb, kx, W, bi, sqs, rss, nfeat=D):
        pss = self.P5 if bi % 2 == 0 else self.P6; kps = 'P5' if bi % 2 == 0 else 'P6'
        rs = rss[bi % 2]; krs = 'rs%d' % (bi % 2)
        for c in range(KC):
            sqc = sqs[c % 2]; ksqc = 'sq%d' % (c % 2)
            self.ACT(sqc[:, :W], xb[:, c, :W], AF.Square, [kx], [ksqc])
            self.MM(pss[:, :W], self.onesb[:, 0:128], sqc[:, :W], c == 0, c == KC - 1, [ksqc, 'onesb'], [kps])
        self.rstd_from_ps(pss[:, :W], kps, rs[:, :W], krs, W, nfeat)
        return rs, krs

    def phase_norm1(self, l):
        with self.scope() as S:
            xbs = [S.sb("n1_xb%d" % i, [128, KC, 512], F32) for i in range(2)]
            sqs = [S.sb("n1_sq%d" % i, [128, 512], BF16) for i in range(2)]
            rss = [S.sb("n1_rs%d" % i, [128, 512], F32) for i in range(2)]
            tfs = [S.sb("n1_tf%d" % i, [128, 512], F32) for i in range(2)]
            for bi, (t0, W) in enumerate(BLOCKS):
                j = 1 if bi == 0 else 0
                xb = xbs[bi % 2]; kx = 'xb%d' % (bi % 2)
                self.DMA('sp', xb[:, :, :W], self.xT[:, :, t0:t0 + W], [('xT', bi)], [kx])
                rs, krs = self.norm_block(S, xb, kx, W, bi, sqs, rss)
                for c in range(KC):
                    tf = tfs[c % 2]; ktf = 'tf%d' % (c % 2)
                    self.TT('dve', tf[:, :W], xb[:, c, :W], rs[:, :W], ALU.mult, [kx, krs], [ktf])
                    self.ACT(self.hT[:, c, t0:t0 + W], tf[:, :W], AF.Identity, [ktf, 'G1', 'modv'], [('hT', bi)],
                             scale=self.G1[:, c, j:j + 1], bias=self.modv[:, c, j:j + 1])

    def proj(self, ps, kps, w, kw, c0, bi, W, t0):
        for kc in range(KC):
            self.MM(ps[:, :W], w[:, kc, c0:c0 + 128], self.hT[:, kc, t0:t0 + W], kc == 0, kc == KC - 1, [kw, ('hT', bi)], [kps])

    def qknorm_rope(self, ps, kps, gcol, W, lat, cs, kcs, tf, out_ap, kout, tb=None):
        (pS, kS), (pW, kW) = tb if tb is not None else ((self.P5, 'P5'), (self.P6, 'P6'))
        self.ACT(tf[0][:, :W], ps[:, :W], AF.Square, [kps], ['qt0'])
        self.MM(pS[:, :W], self.blk64[:], tf[0][:, :W], True, True, ['qt0', 'blk64'], [kS])
        self.rstd_from_ps(pS[:, :W], kS, tf[1][:, :W], 'qt1', W, 64)
        self.STT(tf[2][:, :W], ps[:, :W], gcol, tf[1][:, :W], ALU.mult, ALU.mult, [kps, 'qt1', 'vec2'], ['qt2'])
        if lat:
            self.MM(pW[:, :W], self.swapP[:], tf[2][:, :W], True, True, ['qt2', 'swapP'], [kW])
            self.TT('pool', tf[3][:, :W], tf[2][:, :W], cs[0][:, :W], ALU.mult, ['qt2', kcs], ['qt3'])
            self.TT('dve', tf[0][:, :W], pW[:, :W], cs[1][:, :W], ALU.mult, [kW, kcs], ['qt0'])
            self.TT('dve', out_ap, tf[3][:, :W], tf[0][:, :W], ALU.add, ['qt3', 'qt0'], [kout])
        else:
            self.CP('act', out_ap, tf[2][:, :W], ['qt2'], [kout])

    def phase_attn(self, l):
        last = (l == DEPTH - 1)
        with self.scope('attn') as S:
            wk = S.sb("at_wk", [128, KC, 512], BF16); wv = S.sb("at_wv", [128, KC, 256], BF16); wq = S.sb("at_wq", [128, KC, 1024], BF16)
            self.wload(wk, self.wk_dup[l], 'wk'); self.wload(wv, self.w_in[l], 'wv', O_AV, O_AV + 256); self.wload(wq, self.w_in[l], 'wq', O_AQ, O_AQ + 1024)
            KT = S.sb("at_KT", [128, T], BF16); VA = S.sb("at_VA", [128, NT, 128], BF16)
            css = [[S.sb("at_cs%d%d" % (i, k), [128, 512], F32) for k in range(2)] for i in range(2)]
            tfs = [[S.sb("at_tf%d_%d" % (s_, i), [128, 512], F32) for i in range(4)] for s_ in range(2)]
            QN = [S.sb("at_QN%d" % i, [128, T], BF16) for i in range(2)]
            pts = [S.sb("at_pt%d" % i, [128, 1024], BF16) for i in range(3)]
            oacp = [[S.sb("at_oc%d%d" % (i, h), [128, 512], F32) for h in range(2)] for i in range(2)]
            rc = S.sb("at_rc", [64, 512], F32)
            ost = [S.sb("at_os%d" % i, [128, 512], BF16) for i in range(2)]
            self.MS('dve', VA[:, :, 64:128], 1.0, ['VA'])
            st = dict(ncs=0, item=0)
            SPs = ((self.P01, ['P0', 'P1']), (self.P23, ['P2', 'P3']), (self.P45, ['P4', 'P5']))
            OAs = ((self.P6, 'P6'), (self.P7, 'P7'))

            psets = (((self.P0, 'P0'), (self.P1, 'P1'), (self.P2, 'P2')), ((self.P3, 'P3'), (self.P4, 'P4'), (self.P5, 'P5')))

            def prep_gen(w, kw, c0, gcol, bi, out_ap, kout, s_):
                t0, W = BLOCKS[bi]
                lat = bi > 0
                tf = tfs[s_]; q = lambda n_: 'qt%d_%d' % (n_, s_)
                (pp, kpp), (pS, kS), (pW, kW) = psets[s_]
                cs = css[s_]; kcs = 'cs%d' % s_
                if lat:
                    self.DMA('sp', cs[0][:], self.c_cos[:, t0 - TC:t0 - TC + 512], [], [kcs])
                    self.DMA('sp', cs[1][:], self.c_sin[:, t0 - TC:t0 - TC + 512], [], [kcs])
                self.proj(pp, kpp, w, kw, c0, bi, W, t0)
                self.ACT(tf[0][:, :W], pp[:, :W], AF.Square, [kpp], [q(0)])
                self.MM(pS[:, :W], self.blk64[:], tf[0][:, :W], True, True, [q(0), 'blk64'], [kS])
                yield
                self.ACT(tf[1][:, :W], pS[:, :W], AF.Sqrt, [kS], [q(1)], scale=1.0 / 64, bias=EPS)
                yield
                self.RCP(tf[1][:, :W], tf[1][:, :W], [q(1)], [q(1)])
                self.STT(tf[2][:, :W], pp[:, :W], gcol, tf[1][:, :W], ALU.mult, ALU.mult, [kpp, q(1), 'vec2'], [q(2)])
                yield
                if lat:
                    self.MM(pW[:, :W], self.swapP[:], tf[2][:, :W], True, True, [q(2), 'swapP'], [kW])
                    self.TT('pool', tf[3][:, :W], tf[2][:, :W], cs[0][:, :W], ALU.mult, [q(2), kcs], [q(3)])
                    self.TT('dve', tf[0][:, :W], pW[:, :W], cs[1][:, :W], ALU.mult, [kW, kcs], [q(0)])
                    self.TT('dve', out_ap, tf[3][:, :W], tf[0][:, :W], ALU.add, [q(3), q(0)], [kout])
                else:
                    self.CP('act', out_ap, tf[2][:, :W], [q(2)], [kout])
                yield

            def run_lock(gens):
                gens = list(gens)
                while gens:
                    for g_ in list(gens):
                        try:
                            next(g_)
                        except StopIteration:
                            gens.remove(g_)

            def prep_many(jobs):
                for j0 in range(0, len(jobs), 2):
                    run_lock([prep_gen(*job, s_) for s_, job in enumerate(jobs[j0:j0 + 2])])

            for g in range(4):
                prep_many([(wk, 'wk', g * 128, self.vec2[:, 33:34], bi, KT[:, BLOCKS[bi][0]:BLOCKS[bi][0] + BLOCKS[bi][1]], ('KT', bi))
                           for bi in range(len(BLOCKS))])
                for tb_ in range(0, NT, 8):
                    n = min(8, NT - tb_)
                    for j in range(n):
                        tt = tb_ + j
                        for kc in range(KC):
                            self.MM(self.P3[:, j * 64:(j + 1) * 64], self.hT[:, kc, tt * 128:(tt + 1) * 128], wv[:, kc, g * 64:(g + 1) * 64],
                                    kc == 0, kc == KC - 1, ['wv', ('hT', self.blk_of(tt))], ['P3'])
                    self.CP('act', VA[:, tb_:tb_ + n, 0:64], self.P3[:, 0:n * 64].rearrange("p (j e) -> p j e", e=64), ['P3'], ['VA'])
                for qc in (2 * g, 2 * g + 1):
                    qn_all = QN[qc % 2]
                    blks = [bi for bi in range(len(BLOCKS)) if not (bi == 0 and last)]
                    prep_many([(wq, 'wq', qc * 128, self.vec2[:, 32:33], bi, qn_all[:, BLOCKS[bi][0]:BLOCKS[bi][0] + BLOCKS[bi][1]], ('qn', qc % 2, bi))
                               for bi in blks])
                    for _ in range(14):
                        self.MM(self.P6[:, :], self.identb[:], self.onesb[:, :], True, True, ['identb', 'onesb'], ['P6'])
                    for bi in blks:
                        t0, W = BLOCKS[bi]
                        lat = bi > 0
                        kqn = ('qn', qc % 2, bi)
                        kts = list(range(NT)) if lat else [0, 1]
                        it = st['item']; st['item'] += 1
                        osb = ost[it % 2]; kos = 'os%d' % (it % 2)

                        def issue_S(i):
                            kt = kts[i]; SP, ksp = SPs[i % 3]
                            for hf in range(2):
                                o = hf * 64
                                self.MM(SP[:, hf * 512:hf * 512 + W], KT[o:o + 64, kt * 128:(kt + 1) * 128], qn_all[o:o + 64, t0:t0 + W], True, True,
                                        [('KT', self.blk_of(kt)), kqn], [ksp[hf]])

                        issue_S(0)
                        if len(kts) > 1:
                            issue_S(1)
                        for i, kt in enumerate(kts):
                            SP, ksp = SPs[i % 3]
                            pt = pts[i % 3]; kpt = 'pt%d' % (i % 3)
                            self.ACT(pt[:, :].rearrange("p (n w) -> p n w", w=512)[:, :, :W],
                                     SP[:, :].rearrange("p (n w) -> p n w", w=512)[:, :, :W], AF.Exp, ksp, [kpt], scale=0.125)
                            if i + 2 < len(kts):
                                issue_S(i + 2)
                            for hf in range(2):
                                OA, koa = OAs[hf]
                                self.MM(OA[:, :W], VA[:, kt, :], pt[:, hf * 512:hf * 512 + W], i == 0, i == len(kts) - 1, ['VA', kpt], [koa])
                        for hf in range(2):
                            OA, koa = OAs[hf]
                            self.CP('dve', oacp[it % 2][hf][:, :W], OA[:, :W], [koa], [('oacp', it % 2, hf)])
                        for hf in range(2):
                            o = hf * 64
                            oc_ = oacp[it % 2][hf]; koc = ('oacp', it % 2, hf)
                            self.RCP(rc[:, :W], oc_[64:128, :W], [koc], ['rc'])
                            self.TT('dve', osb[o:o + 64, :W], oc_[0:64, :W], rc[:, :W], ALU.mult, [koc, 'rc'], [kos])
                        self.DMA('sp', self.oc[:, qc, t0:t0 + W], osb[:, :W], [kos], [('oc', qc, bi)])

    def blk_of(self, tt):
        tok = tt * 128
        return 0 if tok < TC else 1 + (tok - TC) // 512

    def phase_conv(self, l):
        last = (l == DEPTH - 1)
        with self.scope() as S:
            wB = S.sb("cv_wB", [128, KC, 1024], BF16); wC = S.sb("cv_wC", [128, KC, 1024], BF16); wX = S.sb("cv_wX", [128, KC, 1024], BF16)
            self.wload(wB, self.w_in[l], 'wB', O_CB, O_CB + 1024); self.wload(wC, self.w_in[l], 'wC', O_CC, O_CC + 1024)
            self.wload(wX, self.w_in[l], 'wX', O_CX, O_CX + 1024)
            Uc = S.sb("cv_Uc", [128, TC + 2], F32); Ul = S.sb("cv_Ul", [128, TL + 2], F32)
            dg = S.sb("cv_dg", [128, 3, 128], F32)
            tf = [S.sb("cv_tf%d" % i, [128, 512], F32) for i in range(2)]
            ost = [S.sb("cv_os%d" % i, [128, 512], BF16) for i in range(2)]
            for U, n in ((Uc, TC), (Ul, TL)):
                self.MS('dve', U[:, 0:1], 0.0, ['U']); self.MS('dve', U[:, n + 1:n + 2], 0.0, ['U'])
            for cc in range(8):
                for k in range(3):
                    self.TS('dve', dg[:, k, :], self.ident[:], self.vec2[:, 8 + k * 8 + cc:9 + k * 8 + cc], ALU.mult, ['ident', 'vec2'], ['dg'])
                for bi, (t0, W) in enumerate(BLOCKS):
                    if bi == 0 and last:
                        continue
                    U, tl = (Uc, t0) if bi == 0 else (Ul, t0 - TC)
                    self.proj(self.P4, 'P4', wC, 'wC', cc * 128, bi, W, t0)
                    self.proj(self.P5, 'P5', wX, 'wX', cc * 128, bi, W, t0)
                    self.CP('act', tf[0][:, :W], self.P5[:, :W], ['P5'], ['ctf0'])
                    self.TT('dve', U[:, 1 + tl:1 + tl + W], self.P4[:, :W], tf[0][:, :W], ALU.mult, ['P4', 'ctf0'], [('U', bi)])
                for bi, (t0, W) in enumerate(BLOCKS):
                    if bi == 0 and last:
                        continue
                    U, tl = (Uc, t0) if bi == 0 else (Ul, t0 - TC)
                    rk = [('U', b) for b in range(9)] + ['U', 'dg']
                    for k in range(3):
                        self.MM(self.P6[:, :W], dg[:, k, :], U[:, tl + k:tl + k + W], k == 0, k == 2, rk, ['P6'])
                    self.proj(self.P4, 'P4', wB, 'wB', cc * 128, bi, W, t0)
                    self.CP('act', tf[1][:, :W], self.P6[:, :W], ['P6'], ['ctf1'])
                    osb = ost[bi % 2]; kos = 'cos%d' % (bi % 2)
                    self.TT('dve', osb[:, :W], self.P4[:, :W], tf[1][:, :W], ALU.mult, ['P4', 'ctf1'], [kos])
                    self.DMA('sp', self.ob[:, cc, t0:t0 + W], osb[:, :W], [kos], [('ob', cc, bi)])

    def phase_hgrn(self, l):
        last = (l == DEPTH - 1)
        with self.scope() as S:
            wi = S.sb("hg_wi", [128, KC, 1024], BF16)
            self.wload(wi, self.w_in[l], 'hwi', O_HI, O_HI + 1024)
            vst = [S.sb("hg_vst%d" % i, [64, 1024], BF16) for i in range(2)]
            for tk in range(T // 64):
                st = vst[tk % 2]; kst = 'vst%d' % (tk % 2)
                bi = self.blk_of(tk // 2)
                for hf in range(2):
                    ps, kps = ((self.P4, 'P4'), (self.P5, 'P5'))[hf]
                    for kc in range(KC):
                        self.MM(ps[0:64, :], self.hT[:, kc, tk * 64:(tk + 1) * 64], wi[:, kc, hf * 512:(hf + 1) * 512], kc == 0, kc == KC - 1,
                                ['hwi', ('hT', bi)], [kps])
                    self.CP('act' if hf else 'dve', st[:, hf * 512:(hf + 1) * 512], ps[0:64, :], [kps], [kst])
                self.DMA('sp', self.vtok[tk], st[:], [kst], [('vtok', tk)])
        NCH = 4
        ALLPA = [('PA', c_) for c_ in range(NCH)]; ALLPU = [('PU', c_) for c_ in range(NCH)]
        with self.scope() as S:
            wts = [S.sb("hg_w%d" % f, [128, KC, 256], BF16) for f in range(3)]
            AMall = S.sb("hg_AMall", [128, NCH, 4, 32], BF16)
            ch = []
            for c in range(NCH):
                ch.append(dict(
                    tf=[S.sb("hg_tf%d_%d" % (c, i), [128, 512], F32) for i in range(4)],
                    q32=S.sb("hg_q32_%d" % c, [128, 512], F32),
                    QP=[S.sb("hg_QP%d_%d" % (c, i), [128, 512], BF16) for i in range(2)],
                    KP=[S.sb("hg_KP%d_%d" % (c, i), [128, 512], BF16) for i in range(2)],
                    KGT=[S.sb("hg_KGT%d_%d" % (c, i), [128, 4, 128], BF16) for i in range(2)],
                    DEC=[S.sb("hg_DEC%d_%d" % (c, i), [128, 16], F32) for i in range(2)],
                    KG=S.sb("hg_KG%d" % c, [128, 512], BF16), VH=S.sb("hg_VH%d" % c, [128, 4, 128], BF16),
                    VHm=S.sb("hg_VHm%d" % c, [128, 4, 4, 128], BF16), AM=AMall[:, c],
                    S32=S.sb("hg_S32_%d" % c, [128, 128], F32), Sb=S.sb("hg_Sb%d" % c, [128, 128], BF16),
                    ost=S.sb("hg_ost%d" % c, [128, 512], F32),
                    PO=(self.P0, self.P1, self.P2, self.P3)[c], kPO='P%d' % c,
                    PA=self.P4[:, c * CH:(c + 1) * CH], kPA=('PA', c), PU=self.P5[:, c * 128:(c + 1) * 128], kPU=('PU', c)))

            def mk(c, hd, d, w, kw, hloc):
                C = ch[c]; tf = C['tf']; K = lambda s: (s, c); K2 = lambda s, par: (s, c, par)
                lbc = self.lb[:, l * 16 + d * 8 + hd:l * 16 + d * 8 + hd + 1]
                omc = self.oml[:, l * 16 + d * 8 + hd:l * 16 + d * 8 + hd + 1]
                mask = self.maskF if d == 0 else self.maskB
                dst = self.ofd if d == 0 else self.obd

                def prep_gen(bi, par):
                    t0, W = BLOCKS[bi]
                    n = W // CH
                    QP = C['QP'][par]; KP = C['KP'][par]; KGT = C['KGT'][par]; DEC = C['DEC'][par]
                    self.proj(self.P6, 'P6', w[0], kw[0], hloc * 128, bi, W, t0)
                    self.CP('act', C['q32'][:, :W], self.P6[:, :W], ['P6'], [K('q32')])
                    yield
                    self.proj(self.P6, 'P6', w[1 + d], kw[1 + d], hloc * 128, bi, W, t0)
                    self.ACT(tf[0][:, :W], self.P6[:, :W], AF.Sigmoid, ['P6'], [K('t0')])
                    self.TS('dve', tf[0][:, :W], tf[0][:, :W], omc, ALU.mult, [K('t0'), 'oml', 'lb'], [K('t0')], s2=lbc, op1=ALU.add)
                    yield
                    self.ACT(tf[1][:, :W], tf[0][:, :W], AF.Ln, [K('t0')], [K('t1')])
                    self.ACT(tf[2][:, :W], tf[0][:, :W], AF.Identity, [K('t0')], [K('t2')], scale=-1.0, bias=1.0)
                    yield
                    Bt = tf[3]
                    if d == 0:
                        self.kb.op('dve', [K('t1'), 'mask01'], [K('t3')], lambda: self.nc.vector.tensor_tensor_scan(
                            Bt[:, :W], self.mask01[:, :W], tf[1][:, :W], 0.0, ALU.mult, ALU.add))
                    else:
                        self.kb.op('dve', [K('t1'), 'mask01'], [K('t3')], lambda: self.nc.vector.tensor_tensor_scan(
                            Bt[:, :W][:, ::-1], self.mask01[:, :W], tf[1][:, :W][:, ::-1], 0.0, ALU.mult, ALU.add))
                    Bv = Bt[:, :W].rearrange("p (n c) -> p n c", c=CH)
                    bl = Bv[:, :, CH - 1:CH] if d == 0 else Bv[:, :, 0:1]
                    yield
                    self.ACT(tf[0][:, :W], Bt[:, :W], AF.Exp, [K('t3')], [K('t0')])
                    self.ACT(tf[1][:, :W], Bt[:, :W], AF.Exp, [K('t3')], [K('t1')], scale=-1.0)
                    self.ACT(DEC[:, 0:n], bl.rearrange("p n c -> p (n c)"), AF.Exp, [K('t3')], [K2('DEC', par)])
                    self.TT('dve', QP[:, :W], C['q32'][:, :W], tf[0][:, :W], ALU.mult, [K('q32'), K('t0')], [K2('QP', par)])
                    self.TT('pool', KP[:, :W], tf[2][:, :W], tf[1][:, :W], ALU.mult, [K('t2'), K('t1')], [K2('KP', par)])
                    yield
                    self.TT('dve', tf[0][:, :W].rearrange("p (n c) -> p n c", c=CH), bl.broadcast_to([128, n, CH]), Bv, ALU.subtract, [K('t3')], [K('t0')])
                    self.ACT(tf[0][:, :W], tf[0][:, :W], AF.Exp, [K('t0')], [K('t0')])
                    self.TT('pool', C['KG'][:, :W], tf[2][:, :W], tf[0][:, :W], ALU.mult, [K('t2'), K('t0')], [K('KG')])
                    yield
                    for j in range(W // 128):
                        self.TR(self.PT[:, j * 128:(j + 1) * 128], C['KG'][:, j * 128:(j + 1) * 128], self.identb[:], [K('KG'), 'identb'], ['PT'])
                    self.CP('act', KGT[:, 0:W // 128, :], self.PT[:, 0:W].rearrange("p (j e) -> p j e", e=128), ['PT'], [K2('KGT', par)])
                    yield

                def step_gen(bi, par):
                    t0, W = BLOCKS[bi]
                    n = W // CH
                    QP = C['QP'][par]; KP = C['KP'][par]; KGT = C['KGT'][par]; DEC = C['DEC'][par]
                    kQP, kKP, kKGT, kDEC = K2('QP', par), K2('KP', par), K2('KGT', par), K2('DEC', par)
                    self.DMA('sp', C['VH'][:, 0:W // 128, :],
                             self.vtok[t0 // 64:(t0 + W) // 64, :, hd * 128:(hd + 1) * 128].rearrange("(j two) p e -> (two p) j e", two=2),
                             [('vtok', tk) for tk in range(t0 // 64, (t0 + W) // 64)], [K('VH')])
                    for q_ in range(4):
                        if q_ < 2:
                            self.ACT(C['VHm'][:, 0:W // 128, q_, :], C['VH'][:, 0:W // 128, :], AF.Identity, [K('VH'), 'rowm'], [(K('VHm'), q_)],
                                     scale=self.rowm[:, q_:q_ + 1])
                        else:
                            self.TS('dve', C['VHm'][:, 0:W // 128, q_, :], C['VH'][:, 0:W // 128, :], self.rowm[:, q_:q_ + 1], ALU.mult,
                                    [K('VH'), 'rowm'], [(K('VHm'), q_)])
                    yield
                    cis = list(range(n)) if d == 0 else list(range(n - 1, -1, -1))

                    def emitA(ci):
                        t = ci * CH; T0 = (t // 128) * 128
                        self.MM(C['PA'], KP[:, T0:T0 + 128], QP[:, t:t + CH], True, True, [kKP, kQP], [C['kPA']])

                    def emitM(ci):
                        if c >= 2:
                            return
                        t = ci * CH; q = (t % 128) // CH
                        rows = slice(q * CH, (q + 1) * CH)
                        self.TT('dve', AMall[rows, c:c + 3:2, q, :],
                                self.P4[rows, 0:NCH * CH].rearrange("p (c x) -> p c x", x=CH)[:, c:c + 3:2, :],
                                mask[rows, :][:, None, :].broadcast_to([CH, 2, CH]), ALU.mult,
                                ALLPA + ['maskF', 'maskB'], [(('AM', c), q), (('AM', c + 2), q)])

                    emitA(cis[0])
                    yield
                    emitM(cis[0])
                    yield
                    for idx, ci in enumerate(cis):
                        t = ci * CH; tt = t // 128; q = (t % 128) // CH
                        if idx + 1 < len(cis):
                            emitA(cis[idx + 1])
                        yield
                        if idx + 1 < len(cis):
                            emitM(cis[idx + 1])
                        yield
                        self.MM(C['PU'], KGT[:, tt, :], C['VHm'][:, tt, q, :], True, True, [kKGT, (K('VHm'), q)], [C['kPU']])
                        yield
                        self.MM(C['PO'][:, t:t + CH], C['VH'][:, tt, :], C['AM'][:, q, :], True, False, [K('VH'), (K('AM'), q)], [C['kPO']])
                        self.MM(C['PO'][:, t:t + CH], C['Sb'][:, :], QP[:, t:t + CH], False, True, [K('Sb'), kQP], [C['kPO']])
                        yield
                        self.STT(C['S32'][:, :], C['S32'][:, :], DEC[:, ci:ci + 1], C['PU'], ALU.mult, ALU.add, [K('S32'), kDEC] + ALLPU, [K('S32')])
                        yield
                        self.CP('act', C['Sb'][:, :], C['S32'][:, :], [K('S32')], [K('Sb')])
                        yield
                    if not (bi == 0 and last):
                        self.CP('act', C['ost'][:, :W], C['PO'][:, :W], [C['kPO']], [K('ost')])
                        self.DMA('sp', dst[:, hd, t0:t0 + W], C['ost'][:, :W], [K('ost')], [('ofb', d, hd, bi)])
                    yield

                return prep_gen, step_gen

            def advance(gens):
                for g_ in list(gens):
                    try:
                        next(g_)
                    except StopIteration:
                        gens.remove(g_)

            for hp in range(4):
                kw = ['hgw%d' % f for f in range(3)]
                for f, off in enumerate((O_HQ, O_HF, O_HB)):
                    self.wload(wts[f], self.w_in[l], kw[f], off + hp * 256, off + hp * 256 + 256)
                self.MS('dve', AMall[:], 0.0, [(('AM', c_), q_) for c_ in range(NCH) for q_ in range(4)])
                for _ in range(14):
                    self.MM(self.P6[:, :], self.identb[:], self.onesb[:, :], True, True, ['identb', 'onesb'], ['P6'])
                fns = []; orders = []
                for hl in range(2):
                    for d in range(2):
                        c = hl * 2 + d
                        self.MS('dve', ch[c]['S32'][:], 0.0, [('S32', c)]); self.MS('dve', ch[c]['Sb'][:], 0.0, [('Sb', c)])
                        fns.append(mk(c, hp * 2 + hl, d, wts, kw, hl))
                        orders.append(list(range(9)) if d == 0 else [0] + list(range(8, 0, -1)))
                nb = len(orders[0])
                gens = [fns[c][0](orders[c][0], 0) for c in range(NCH)]
                while gens:
                    advance(gens)
                for i in range(nb):
                    steps = [fns[c][1](orders[c][i], i % 2) for c in range(NCH)]
                    nxt = [fns[c][0](orders[c][i + 1], (i + 1) % 2) for c in range(NCH)] if i + 1 < nb else []
                    nsub = 4 + (BLOCKS[orders[0][i]][1] // CH) * 6
                    period = max(1, nsub // 9)
                    k = 0
                    while steps:
                        advance(steps)
                        k += 1
                        if nxt and k % period == 0:
                            advance(nxt)
                    while nxt:
                        advance(nxt)
        with self.scope() as S:
            wgt = S.sb("hg_wg", [128, KC, 1024], BF16)
            self.wload(wgt, self.w_in[l], 'hwg', O_HG, O_HG + 1024)
            G = 3
            ins = [[S.sb("hg_in%d%d" % (i, k), [128, 512], F32) for k in range(2)] for i in range(G)]
            tfr = [[S.sb("hg_rtf%d_%d" % (i, k), [128, 512], F32) for k in range(3)] for i in range(G)]
            sqbs = [S.sb("hg_sq%d" % i, [128, 512], BF16) for i in range(G)]; ost = [S.sb("hg_os%d" % i, [128, 512], BF16) for i in range(G)]
            ssb = ((self.P0, 'P0'), (self.P1, 'P1'), (self.P2, 'P2')); gbk = ((self.P3, 'P3'), (self.P4, 'P4'), (self.P5, 'P5'))

            def ro_gen(hd, bi, s_):
                t0, W = BLOCKS[bi]
                i2 = ins[s_]; ki = [('hin', s_, k) for k in range(2)]; tf = tfr[s_]; sqb = sqbs[s_]; osb = ost[s_]
                r = lambda n_: ('rr', n_, s_)
                (PS_, kPS), (PG, kPG) = ssb[s_], gbk[s_]
                self.DMA('sp', i2[0][:, :W], self.ofd[:, hd, t0:t0 + W], [], [ki[0]])
                self.DMA('sp', i2[1][:, :W], self.obd[:, hd, t0:t0 + W], [], [ki[1]])
                o = tf[0]
                self.TT('pool', o[:, :W], i2[0][:, :W], i2[1][:, :W], ALU.add, ki, [r(0)])
                self.ACT(sqb[:, :W], o[:, :W], AF.Square, [r(0)], [r(3)])
                self.MM(PS_[:, :W], self.onesb[:, 0:128], sqb[:, :W], True, True, [r(3), 'onesb'], [kPS])
                self.proj(PG, kPG, wgt, 'hwg', hd * 128, bi, W, t0)
                yield
                self.ACT(tf[1][:, :W], PS_[:, :W], AF.Sqrt, [kPS], [r(1)], scale=1.0 / 128, bias=EPS)
                yield
                self.RCP(tf[1][:, :W], tf[1][:, :W], [r(1)], [r(1)])
                self.STT(tf[2][:, :W], o[:, :W], self.vec2[:, hd:hd + 1], tf[1][:, :W], ALU.mult, ALU.mult, [r(0), r(1), 'vec2'], [r(2)])
                yield
                self.ACT(tf[1][:, :W], PG[:, :W], AF.Silu, [kPG, r(1)], [r(1)])
                yield
                self.TT('dve', osb[:, :W], tf[2][:, :W], tf[1][:, :W], ALU.mult, [r(2), r(1)], [('ros', s_)])
                self.DMA('sp', self.oa[:, hd, t0:t0 + W], osb[:, :W], [('ros', s_)], [('oa', hd, bi)])
                yield

            items = [(hd, bi) for hd in range(8) for bi in range(len(BLOCKS)) if not (bi == 0 and last)]
            for g0 in range(0, len(items), G):
                gens = [ro_gen(hd, bi, s_) for s_, (hd, bi) in enumerate(items[g0:g0 + G])]
                while gens:
                    for g_ in list(gens):
                        try:
                            next(g_)
                        except StopIteration:
                            gens.remove(g_)

    def phase_gates(self, l):
        last = (l == DEPTH - 1)
        with self.scope() as S:
            ws = [S.sb("gt_w%d" % i, [128, KC, 1024], BF16) for i in range(3)]
            ost = [S.sb("gt_os%d" % i, [128, 512], BF16) for i in range(2)]
            for f, (off, dst) in enumerate(((O_GA, self.ga), (O_GB, self.gb), (O_GC, self.gc))):
                self.wload(ws[f], self.w_in[l], 'gw%d' % f, off, off + 1024)
                cnt = 0
                for oc in range(8):
                    for bi, (t0, W) in enumerate(BLOCKS):
                        if bi == 0 and last:
                            continue
                        ps, kps = ((self.P4, 'P4'), (self.P5, 'P5'))[cnt % 2]
                        osb = ost[cnt % 2]; kos = 'gos%d' % (cnt % 2); cnt += 1
                        self.proj(ps, kps, ws[f], 'gw%d' % f, oc * 128, bi, W, t0)
                        self.ACT(osb[:, :W], ps[:, :W], AF.Sigmoid, [kps], [kos])
                        self.DMA('sp', dst[:, oc, t0:t0 + W], osb[:, :W], [kos], [('g', f, oc, bi)])

    def phase_merge(self, l):
        last = (l == DEPTH - 1)
        with self.scope() as S:
            wa = S.sb("mg_wa", [128, KC, 1024], BF16); wb = S.sb("mg_wb", [128, KC, 1024], BF16)
            wc = S.sb("mg_wc", [128, KC, 1024], BF16); wo = S.sb("mg_wo", [128, KC, 1024], BF16)
            self.wload(wa, self.wpa[l], 'wa'); self.wload(wb, self.wpb[l], 'wb'); self.wload(wc, self.wpc[l], 'wc'); self.wload(wo, self.wout[l], 'wo')
            st = [[S.sb("mg_s%d%d" % (i, k), [128, KC, 256], BF16) for k in range(6)] for i in range(2)]
            xbs = [S.sb("mg_xb%d" % i, [128, KC, 256], F32) for i in range(2)]
            yb = [S.sb("mg_yb%d" % i, [128, KC, 256], BF16) for i in range(2)]
            tfm = [[S.sb("mg_tf%d_%d" % (s_, i), [128, 256], F32) for i in range(3)] for s_ in range(2)]
            srcs = (self.oa, self.ob, self.oc, self.ga, self.gb, self.gc)
            W = 256
            for bi, (t0, _) in enumerate(B256):
                if bi == 0 and last:
                    continue
                j = 1 if bi == 0 else 0
                s = st[bi % 2]; ks = ['ms%d%d' % (bi % 2, k) for k in range(6)]
                for k in range(6):
                    self.DMA('sp', s[k][:], srcs[k][:, :, t0:t0 + W], [], [ks[k]])
                xb = xbs[bi % 2]; kx = 'mxb%d' % (bi % 2)
                self.DMA('sp', xb[:], self.xT[:, :, t0:t0 + W], [], [kx])
                y = yb[bi % 2]; ky = 'myb%d' % (bi % 2)
                for oc in range(8):
                    so = oc % 2
                    tf = tfm[so]; kt = ['mtf%d_%d' % (k_, so) for k_ in range(3)]
                    banks = (((self.P0, 'P0'), (self.P1, 'P1'), (self.P2, 'P2')), ((self.P3, 'P3'), (self.P4, 'P4'), (self.P5, 'P5')))[so]
                    for k, (w, kw) in enumerate(((wa, 'wa'), (wb, 'wb'), (wc, 'wc'))):
                        ps, kps = banks[k]
                        for kc in range(KC):
                            self.MM(ps[:, :W], w[:, kc, oc * 128:(oc + 1) * 128], s[k][:, kc, :], kc == 0, kc == KC - 1, [kw, ks[k]], [kps])
                        self.TT('dve', tf[k][:, :], ps[:, :W], s[3 + k][:, oc, :], ALU.mult, [kps, ks[3 + k]], [kt[k]])
                    self.TT('pool', tf[0][:, :], tf[0][:, :], tf[1][:, :], ALU.add, [kt[0], kt[1]], [kt[0]])
                    self.TT('pool', y[:, oc, :], tf[0][:, :], tf[2][:, :], ALU.add, [kt[0], kt[2]], [(ky, oc)])
                for oc in range(8):
                    ps, kps = ((self.P6, 'P6'), (self.P0, 'P0'))[oc % 2]
                    for kc in range(KC):
                        self.MM(ps[:, :W], wo[:, kc, oc * 128:(oc + 1) * 128], y[:, kc, :], kc == 0, kc == KC - 1, ['wo', (ky, kc)], [kps])
                    self.STT(xb[:, oc, :], ps[:, :W], self.modv[:, 16 + oc, j:j + 1], xb[:, oc, :], ALU.mult, ALU.add, [kps, 'modv', kx], [kx])
                self.DMA('sp', self.xT[:, :, t0:t0 + W], xb[:], [kx], [('xTm', bi)])

    def phase_router(self, l):
        grps = self.groups(l)
        with self.scope() as S:
            xbs = [S.sb("rt_xb%d" % i, [128, KC, 512], F32) for i in range(2)]
            sqs = [S.sb("rt_sq%d" % i, [128, 512], BF16) for i in range(2)]
            rss = [S.sb("rt_rs%d" % i, [128, 512], F32) for i in range(2)]
            tfs = [S.sb("rt_tf%d" % i, [128, 512], F32) for i in range(2)]
            h2f = S.sb("rt_h2f", [128, KC, 512], F32); h2b = S.sb("rt_h2b", [128, KC, 512], BF16)
            rwt = S.sb("rt_rw", [128, KC, NE], F32)
            A = S.sb("rt_A", [NE, T], F32); Bm = S.sb("rt_B", [NE, T], F32); Cp = S.sb("rt_C", [NE, T], F32)
            sv = S.sb("rt_sv", [NE, 8], F32)
            self.DMA('sp', rwt[:], self.rw[l].rearrange("(k p) n -> p k n", p=128), [], ['rwt'])
            for bi, (t0, W) in enumerate(BLOCKS):
                if bi == 0 and len(grps) == 1:
                    continue
                j = 1 if bi == 0 else 0
                xb = xbs[bi % 2]; kx = 'xb%d' % (bi % 2)
                self.DMA('sp', xb[:, :, :W], self.xT[:, :, t0:t0 + W], [], [kx])
                rs, krs = self.norm_block(S, xb, kx, W, bi, sqs, rss)
                for c in range(KC):
                    tf = tfs[c % 2]; ktf = 'tf%d' % (c % 2)
                    self.TT('dve', tf[:, :W], xb[:, c, :W], rs[:, :W], ALU.mult, [kx, krs], [ktf])
                    self.ACT(h2f[:, c, :W], tf[:, :W], AF.Identity, [ktf, 'G2', 'modv'], ['h2f'],
                             scale=self.G2[:, c, j:j + 1], bias=self.modv[:, 24 + c, j:j + 1])
                self.CP('pool', h2b[:, :, :W], h2f[:, :, :W], ['h2f'], ['h2b'])
                for kc in range(KC):
                    self.MM(self.P4[0:NE, :W], rwt[:, kc, :], h2f[:, kc, :W], kc == 0, kc == KC - 1, ['rwt', 'h2f'], ['P4'])
                self.ACT(A[:, t0:t0 + W], self.P4[0:NE, :W], AF.Exp, ['P4'], [('A', bi)])
                self.MM(self.P0[0:NE, :W], self.onesf[0:NE, 0:NE], A[:, t0:t0 + W], True, True, [('A', bi), 'onesf'], ['P0'])
                self.RCP(Bm[:, t0:t0 + W], self.P0[0:NE, :W], ['P0'], [('B', bi)])
                self.TT('dve', A[:, t0:t0 + W], A[:, t0:t0 + W], Bm[:, t0:t0 + W], ALU.mult, [('A', bi), ('B', bi)], [('A', bi)])
                for jt in range(W // 128):
                    tt = t0 // 128 + jt
                    for kc in range(KC):
                        self.TR(self.PT[:, kc * 128:(kc + 1) * 128], h2b[:, kc, jt * 128:(jt + 1) * 128], self.identb[:], ['h2b', 'identb'], ['PT'])
                    self.CP('act' if jt % 2 else 'dve', self.h2t[:, tt, :], self.PT[:, :], ['PT'], [('h2t', tt)])
            allA = [('A', b) for b in range(9)]; allB = [('B', b) for b in range(9)]
            LO, HI, MID, CNT, M, DL, DH = (sv[:, i:i + 1] for i in range(7))
            for g in grps:
                a0, n, cap = g['tok0'], g['ntok'], g['cap']
                Ag = A[:, a0:a0 + n]; Bg = Bm[:, a0:a0 + n]; Cg = Cp[:, a0:a0 + n]
                self.MS('dve', LO, 0.0, ['sv']); self.MS('dve', HI, 1.0, ['sv'])
                for it in range(26):
                    self.TS('dve', MID, LO, HI, ALU.add, ['sv'], ['sv'], s2=0.5, op1=ALU.mult)
                    self.TS('dve', Bg, Ag, MID, ALU.is_gt, allA + ['sv'], allB + ['sv'], s2=None, op1=ALU.add, accum_out=CNT)
                    self.TS('dve', M, CNT, float(cap), ALU.is_ge, ['sv'], ['sv'])
                    self.TT('dve', DL, MID, LO, ALU.subtract, ['sv'], ['sv'])
                    self.TT('dve', DH, HI, MID, ALU.subtract, ['sv'], ['sv'])
                    self.STT(LO, DL, M, LO, ALU.mult, ALU.add, ['sv'], ['sv'])
                    self.STT(HI, DH, M, MID, ALU.mult, ALU.add, ['sv'], ['sv'])
                self.TS('dve', Bg, Ag, LO, ALU.is_gt, allA + ['sv'], allB)
                for c0 in range(0, n, 512):
                    w = min(512, n - c0)
                    init = 0.0 if c0 == 0 else Cg[:, c0 - 1:c0]
                    self.kb.op('dve', allB + ['onesf', 'C'], ['C'], lambda c0=c0, w=w, init=init: self.nc.vector.tensor_tensor_scan(
                        Cg[:, c0:c0 + w], self.onesf[0:NE, 0:1].broadcast_to([NE, w]), Bg[:, c0:c0 + w], init, ALU.mult, ALU.add))
                self.TT('dve', Cg, Cg, Bg, ALU.mult, ['C'] + allB, ['C'])
                self.TS('dve', Cg, Cg, -1.0, ALU.add, ['C'], ['C'])
                self.TT('dve', Ag, Ag, Bg, ALU.mult, allA + allB, allA)
                self.DMA('sp', self.posm_d[:, a0:a0 + n], Cg, ['C'], ['posm_d'])
                self.DMA('sp', self.wm_d[:, a0:a0 + n], Ag, allA, ['wm_d'])
                for tt in range(a0 // 128, (a0 + n) // 128):
                    self.TR(self.P4[:, (tt % 32) * NE:(tt % 32 + 1) * NE], Cp[:, tt * 128:(tt + 1) * 128], self.ident[0:NE, 0:NE], ['C', 'ident'], ['P4'])
                    if tt % 32 == 31 or tt == (a0 + n) // 128 - 1:
                        b0 = (tt // 32) * 32
                        self.CP('dve', self.ptok[:, b0:tt + 1, :], self.P4[:, 0:(tt + 1 - b0) * NE].rearrange("p (t e) -> p t e", e=NE), ['P4'], ['ptok'])

    def phase_moeA(self, l):
        grps = self.groups(l)
        with self.scope() as S:
            wring = [S.sb("ma_w%d" % i, [128, KC, 1024], BF16) for i in range(4)]
            SEL = S.sb("ma_sel", [128, 16, 512], BF16)
            XS = S.sb("ma_xs", [128, KC, 512], BF16); HID = S.sb("ma_hid", [128, KC, 512], BF16)
            YS = [S.sb("ma_ys%d" % i, [128, 4, 1024], BF16) for i in range(2)]
            sg = [S.sb("ma_sg%d" % i, [128, 512], F32) for i in range(2)]
            nsel = 0
            srcw = (self.wg, self.wu, self.wd)

            def slot(e, m):
                return (3 * e + m) % 4

            def load(e, m):
                if e < NE:
                    self.wload(wring[slot(e, m)], srcw[m][l, e], 'mw%d' % slot(e, m))

            for m in range(3):
                load(0, m)
            for e in range(NE):
                wr = [wring[slot(e, m)] for m in range(3)]
                kwr = ['mw%d' % slot(e, m) for m in range(3)]
                load(e + 1, 0)
                for g in grps:
                    cap = g['cap']; tts = list(range(g['tok0'] // 128, (g['tok0'] + g['ntok']) // 128))
                    for h0 in range(0, len(tts), 16):
                        part = tts[h0:h0 + 16]
                        for i, tt in enumerate(part):
                            self.TS('dve', SEL[:, i, :cap], self.iota[:, :cap], self.ptok[:, tt, e:e + 1], ALU.is_equal,
                                    ['iota', 'ptok'], [('sel', i)])
                        for kc in range(KC):
                            ps, kps = ((self.P4, 'P4'), (self.P5, 'P5'))[kc % 2]
                            for i, tt in enumerate(part):
                                self.MM(ps[:, :cap], self.h2t[:, tt, kc * 128:(kc + 1) * 128], SEL[:, i, :cap], i == 0, i == len(part) - 1,
                                        [('sel', i)], [kps])
                            if h0 == 0:
                                self.CP('act', XS[:, kc, :cap], ps[:, :cap], [kps], [('xs', kc)])
                            else:
                                self.TT('dve', XS[:, kc, :cap], ps[:, :cap], XS[:, kc, :cap], ALU.add, [kps, ('xs', kc)], [('xs', kc)])
                    xk = [('xs', kc) for kc in range(KC)]
                    for fc in range(8):
                        for kc in range(KC):
                            self.MM(self.P0[:, :cap], wr[0][:, kc, fc * 128:(fc + 1) * 128], XS[:, kc, :cap], kc == 0, kc == KC - 1, [kwr[0]] + xk, ['P0'])
                        for kc in range(KC):
                            self.MM(self.P1[:, :cap], wr[1][:, kc, fc * 128:(fc + 1) * 128], XS[:, kc, :cap], kc == 0, kc == KC - 1, [kwr[1]] + xk, ['P1'])
                        s_ = sg[fc % 2]; ksg = 'sg%d' % (fc % 2)
                        self.ACT(s_[:, :cap], self.P0[:, :cap], AF.Silu, ['P0'], [ksg])
                        self.TT('dve', HID[:, fc, :cap], self.P1[:, :cap], s_[:, :cap], ALU.mult, ['P1', ksg], [('hid', fc)])
                    hk = [('hid', fc) for fc in range(8)]
                    if g is grps[-1]:
                        load(e + 1, 1)
                    ys = YS[nsel % 2]; kys = 'ys%d' % (nsel % 2); nsel += 1
                    njt = (cap + 127) // 128
                    cnt = 0
                    for jt in range(njt):
                        nj = min(128, cap - jt * 128)
                        for db in range(2):
                            ps, kps = ((self.P2, 'P2'), (self.P3, 'P3'))[cnt % 2]; cnt += 1
                            for fc in range(8):
                                self.MM(ps[0:nj, :], HID[:, fc, jt * 128:jt * 128 + nj], wr[2][:, fc, db * 512:(db + 1) * 512], fc == 0, fc == 7, [kwr[2]] + hk, [kps])
                            self.CP('act' if db else 'dve', ys[0:nj, jt, db * 512:(db + 1) * 512], ps[0:nj, :], [kps], [kys])
                    if g is grps[-1]:
                        load(e + 1, 2)
                    if g['name'] == 'l':
                        self.DMA('sp', self.ysl[e], ys[:], [kys], [('ysl', e)])
                    else:
                        self.DMA('sp', self.ysc[e], ys[0:32, 0, :], [kys], [('ysc', e)])

    def phase_moeB(self, l):
        grps = self.groups(l)
        with self.scope() as S:
            YA = S.sb("mb_ya", [128, NE, 4, 1024], BF16)
            pb = [S.sb("mb_pb%d" % i, [128, 256], F32) for i in range(3)]; wbc = [S.sb("mb_wb%d" % i, [128, 256], F32) for i in range(3)]
            selt = [S.sb("mb_st%d" % i, [128, 4, 256], BF16) for i in range(3)]
            xbs = [S.sb("mb_xb%d" % i, [128, KC, 256], F32) for i in range(2)]
            W = 256
            acc = [(self.P0, 'P0'), (self.P1, 'P1'), (self.P2, 'P2'), (self.P3, 'P3')]
            for g in grps:
                cap = g['cap']; j = g['j']
                njt = (cap + 127) // 128
                if g['name'] == 'l':
                    for e in range(NE):
                        self.DMA('sp', YA[:, e], self.ysl[e], [], ['YA'])
                else:
                    for e in range(NE):
                        self.DMA('sp', YA[0:32, e, 0, :], self.ysc[e], [], ['YA'])
                cnt = 0
                for t0 in range(g['tok0'], g['tok0'] + g['ntok'], W):
                    bi = t0 // W
                    xb = xbs[bi % 2]; kx = 'bxb%d' % (bi % 2)
                    self.DMA('sp', xb[:], self.xT[:, :, t0:t0 + W], [], [kx])
                    for (ps, kps) in acc:
                        self.MM(ps[:, :], self.zerob[:, :], self.onesb[:, :], True, False, ['zerob', 'onesb'], [kps], skip_group_check=True)
                    for e in range(NE):
                        p_ = pb[cnt % 3]; w_ = wbc[cnt % 3]; s_ = selt[cnt % 3]
                        kp_, kw_, ks_ = 'pb%d' % (cnt % 3), 'wb%d' % (cnt % 3), 'st%d' % (cnt % 3); cnt += 1
                        self.DMA('sp', p_[:], self.posm_d[e:e + 1, t0:t0 + W].broadcast_to([128, W]), [], [kp_])
                        self.DMA('sp', w_[:], self.wm_d[e:e + 1, t0:t0 + W].broadcast_to([128, W]), [], [kw_])
                        for jt in range(njt):
                            nj = min(128, cap - jt * 128)
                            self.STT(s_[0:nj, jt, :], p_[0:nj, :], self.jcol[0:nj, jt:jt + 1], w_[0:nj, :], ALU.is_equal, ALU.mult, [kp_, kw_, 'jcol'], [ks_])
                        for c in range(KC):
                            ps, kps = acc[c // 2]
                            for jt in range(njt):
                                nj = min(128, cap - jt * 128)
                                lastmm = (e == NE - 1 and jt == njt - 1)
                                self.MM(ps[:, (c % 2) * W:(c % 2 + 1) * W], YA[0:nj, e, jt, c * 128:(c + 1) * 128], s_[0:nj, jt, :], False, lastmm,
                                        ['YA', ks_], [kps], skip_group_check=True)
                    for c in range(KC):
                        ps, kps = acc[c // 2]
                        self.STT(xb[:, c, :], ps[:, (c % 2) * W:(c % 2 + 1) * W], self.modv[:, 40 + c, j:j + 1], xb[:, c, :], ALU.mult, ALU.add,
                                 [kps, 'modv', kx], [kx])
                    self.DMA('sp', self.xT[:, :, t0:t0 + W], xb[:], [kx], [('xTb', bi)])

    def phase_final(self):
        with self.scope() as S:
            xbs = [S.sb("fn_xb%d" % i, [128, KC, 512], F32) for i in range(2)]
            sqs = [S.sb("fn_sq%d" % i, [128, 512], BF16) for i in range(2)]
            rss = [S.sb("fn_rs%d" % i, [128, 512], F32) for i in range(2)]
            yb = S.sb("fn_y", [128, KC, 512], F32)
            ot = [S.sb("fn_o%d" % i, [128, 1024], F32) for i in range(2)]
            cnt = 0
            for bi, (t0, W) in enumerate(BLOCKS):
                if bi == 0:
                    continue
                xb = xbs[bi % 2]; kx = 'xb%d' % (bi % 2)
                self.DMA('sp', xb[:, :, :W], self.xT[:, :, t0:t0 + W], [], [kx])
                rs, krs = self.norm_block(S, xb, kx, W, bi, sqs, rss)
                for c in range(KC):
                    self.STT(yb[:, c, :W], xb[:, c, :W], self.fngs[:, c:c + 1], rs[:, :W], ALU.mult, ALU.mult, [kx, krs, 'fngs'], [('y', c)])
                for jt in range(W // 128):
                    o = ot[cnt % 2]; ko = 'fo%d' % (cnt % 2); cnt += 1
                    for hf in range(2):
                        ps, kps = ((self.P0, 'P0'), (self.P1, 'P1'))[hf]
                        for c4 in range(4):
                            c = hf * 4 + c4
                            self.TR(ps[:, c4 * 128:(c4 + 1) * 128], yb[:, c, jt * 128:(jt + 1) * 128], self.ident[:], [('y', c), 'ident'], [kps])
                        self.CP('act' if hf else 'dve', o[:, hf * 512:(hf + 1) * 512], ps[:, :], [kps], [ko])
                    r0 = t0 - TC + jt * 128
                    self.DMA('sp', self.out[r0:r0 + 128, :], o[:], [ko], [('out', r0)])

    def debug_dump(self):
        if getattr(self, '_dumped', False):
            return
        self._dumped = True
        with self.scope() as S:
            for name in self.dbg:
                if name == 'hT':
                    o = self.dout("dbg_hT", [128, KC, T], BF16)
                    self.DMA('sp', o, self.hT[:], [], ['dbg_hT'])
                elif name == 'modv':
                    o = self.dout("dbg_modv", [128, 96], F32)
                    self.DMA('sp', o, self.modv[:].rearrange("p m j -> p (m j)"), [], ['dbg_modv'])
                elif name in ('xT', 'oa', 'ob', 'oc', 'ga'):
                    dt = F32 if name == 'xT' else BF16
                    srcd = getattr(self, name)
                    o = self.dout("dbg_" + name, [128, KC, T], dt)
                    xb = S.sb("dbg_xb_" + name, [128, KC, 512], dt)
                    for bi, (t0, W) in enumerate(BLOCKS):
                        self.DMA('sp', xb[:, :, :W], srcd[:, :, t0:t0 + W], [], ['dxb'])
                        self.DMA('sp', o[:, :, t0:t0 + W], xb[:, :, :W], ['dxb'], ['dxo'])

def _consts():
    ident = np.eye(128, dtype=np.float32)
    blk64 = np.zeros((128, 128), np.float32); blk64[:64, :64] = 1; blk64[64:, 64:] = 1
    swap = np.zeros((128, 128), np.float32)
    for j in range(64):
        swap[2 * j + 1, 2 * j] = 1; swap[2 * j, 2 * j + 1] = 1
    s = np.arange(32)
    maskF = np.tile((s[:, None] <= s[None, :]).astype(np.float32), (4, 1))
    maskB = np.tile((s[:, None] >= s[None, :]).astype(np.float32), (4, 1))
    rowm = np.zeros((128, 4), np.float32)
    for q_ in range(4):
        rowm[q_ * 32:(q_ + 1) * 32, q_] = 1.0
    mask01 = np.ones((128, 512), np.float32); mask01[:, 0::32] = 0
    iota = np.tile(np.arange(512, dtype=np.float32)[None, :], (128, 1))
    jcol = (np.arange(128, dtype=np.float32)[:, None] + 128 * np.arange(4, dtype=np.float32)[None, :]).astype(np.float32)
    rows = TL // 64
    row = np.repeat(np.arange(rows), 64).astype(np.float32); col = np.tile(np.arange(64), rows).astype(np.float32)
    inv = (np.float32(10000.0) ** (-np.arange(0, 32, 2, dtype=np.float32) / np.float32(32))).astype(np.float32)
    ang = np.concatenate([row[:, None] * inv, col[:, None] * inv], axis=-1).astype(np.float32)
    cos = np.cos(ang).astype(np.float32); sin = np.sin(ang).astype(np.float32)
    p = np.arange(128); pj = (p % 64) // 2
    cosT = np.ascontiguousarray(cos[:, pj].T)
    sgn = np.where(p % 2 == 0, -1.0, 1.0).astype(np.float32)
    sinS = np.ascontiguousarray((sin[:, pj] * sgn[None, :]).T)
    return dict(c_ident=ident, c_blk64=blk64, c_swap=swap, c_maskF=maskF, c_maskB=maskB, c_rowm=rowm, c_mask01=mask01, c_iota=iota, c_jcol=jcol,
                c_cos=cosT, c_sin=sinS)


def _cols(v):
    return np.ascontiguousarray(np.swapaxes(v.reshape(v.shape[:-1] + (8, 128)), -1, -2))


def prep_inputs(inp):
    shared = dict(_consts())
    f = lambda a: np.ascontiguousarray(np.asarray(a, dtype=np.float32))
    shared['ada_w'] = f(inp['ada_w'])
    shared['ada_b'] = np.ascontiguousarray(np.swapaxes(f(inp['ada_b']).reshape(DEPTH, 48, 128), 1, 2))
    shared['n1g'] = _cols(f(inp['norm1_g'])); shared['n2g'] = _cols(f(inp['norm2_g'])); shared['fng'] = _cols(f(inp['final_norm_g']))
    shared['hgn'] = _cols(f(inp['hg_norm_g']))
    cw = f(inp['conv_w'])
    shared['convw'] = np.ascontiguousarray(np.concatenate([_cols(cw[:, k]) for k in range(3)], axis=-1))
    shared['qng'] = np.ascontiguousarray(np.tile(f(inp['q_norm_g']), (1, 2))[:, :, None])
    shared['kng'] = np.ascontiguousarray(np.tile(f(inp['k_norm_g']), (1, 2))[:, :, None])
    lbl = f(inp['hg_lb_logits'])
    shared['lbl'] = np.ascontiguousarray(_cols(lbl).transpose(2, 0, 1, 3).reshape(128, 32))
    w_in = f(inp['w_in'])
    shared['w_in'] = w_in
    wk = w_in[:, :, O_AK:O_AK + 256]
    shared['wk_dup'] = np.ascontiguousarray(np.concatenate([np.concatenate([wk[:, :, g * 64:(g + 1) * 64]] * 2, axis=-1) for g in range(4)], axis=-1))
    for k in ('w_proj_a', 'w_proj_b', 'w_proj_c', 'w_out', 'router_w', 'w_gate', 'w_up', 'w_down'):
        shared[k] = f(inp[k])
    x = f(inp['x']); ctx = f(inp['ctx']); c = f(inp['c']); c_ctx = f(inp['c_ctx'])
    maps = []
    for b in range(x.shape[0]):
        m = dict(shared)
        m['x'] = x[b]; m['ctx'] = ctx[b]
        m['cc'] = np.ascontiguousarray(np.concatenate([c[b].reshape(8, 128).T, c_ctx.reshape(8, 128).T], axis=1))
        maps.append(m)
    return maps


def kernel(**inputs):
    maps = prep_inputs(inputs)
    prog = Prog()
    nc = prog.build()
    maps = [{k: v for k, v in m.items() if k in prog.inputs} for m in maps]
    res = run_bass_kernel_spmd(nc, maps, core_ids=list(range(8)))
    return np.stack([np.asarray(r["out"], dtype=np.float32) for r in res.results], axis=0)
```

```python
import numpy as np
import concourse.bass as bass
import concourse.mybir as mybir
from concourse.bass_utils import run_bass_kernel_spmd
from contextlib import ExitStack

F32 = mybir.dt.float32
BF16 = mybir.dt.bfloat16
ALU = mybir.AluOpType
AF = mybir.ActivationFunctionType

D = 1024; KC = 8; TC = 256; TL = 4096; T = TC + TL; NT = T // 128
DEPTH = 2; NE = 16; EPS = 1e-6; CH = 32
BLOCKS = [(0, 256)] + [(256 + 512 * i, 512) for i in range(8)]
B256 = [(256 * i, 256) for i in range(17)]
O_HQ, O_HF, O_HB, O_HI, O_HG, O_CB, O_CC, O_CX, O_AQ, O_AK, O_AV, O_GA, O_GB, O_GC = (
    0, 1024, 2048, 3072, 4096, 5120, 6144, 7168, 8192, 9216, 9472, 9728, 10752, 11776)
NDS = 6


class KB:
    def __init__(self, nc, es):
        self.nc = nc
        self.E = {'pe': nc.tensor, 'act': nc.scalar, 'dve': nc.vector, 'pool': nc.gpsimd, 'sp': nc.sync}
        self.esem = {k: es.enter_context(nc.semaphore('e_' + k)) for k in self.E}
        self.ecnt = {k: 0 for k in self.E}
        self.seen = {k: {} for k in self.E}
        self.lastw = {}
        self.readers = {}
        self.dsem = {q: [es.enter_context(nc.semaphore('d_%s%d' % (q, i))) for i in range(NDS)] for q in ('sp', 'pool', 'act')}
        self.dcnt = {q: [0] * NDS for q in self.dsem}
        self.dnext = {q: 0 for q in self.dsem}
        self.ninstr = 0

    def _wait(self, eng, tok):
        key, sem, val = tok
        if self.seen[eng].get(key, 0) >= val:
            return
        self.E[eng].wait_ge(sem, val)
        self.seen[eng][key] = val

    def _deps(self, eng, reads, writes):
        toks = []
        for r in reads:
            t = self.lastw.get(r)
            if t is not None:
                toks.append(t)
        for w in writes:
            t = self.lastw.get(w)
            if t is not None:
                toks.append(t)
            rd = self.readers.get(w)
            if rd:
                toks.extend(rd.values())
        for t in toks:
            if eng == 'pe' and t[0] == 'pe':
                continue
            self._wait(eng, t)

    def _commit(self, tok, reads, writes):
        for r in reads:
            self.readers.setdefault(r, {})[tok[0]] = tok
        for w in writes:
            self.lastw[w] = tok
            self.readers[w] = {}

    def op(self, eng, reads, writes, fn):
        self._deps(eng, reads, writes)
        ins = fn()
        self.ecnt[eng] += 1
        ins.then_inc(self.esem[eng], 1)
        self._commit((eng, self.esem[eng], self.ecnt[eng]), reads, writes)
        self.ninstr += 1

    def dma(self, q, out, in_, reads, writes, **kw):
        self._deps(q, reads, writes)
        i = self.dnext[q]
        self.dnext[q] = (i + 1) % NDS
        sem = self.dsem[q][i]
        key = ('d', q, i)
        if self.dcnt[q][i] > 0:
            self._wait(q, (key, sem, self.dcnt[q][i]))
        self.dcnt[q][i] += 16
        self.E[q].dma_start(out=out, in_=in_, **kw).then_inc(sem, 16)
        self._commit((key, sem, self.dcnt[q][i]), reads, writes)
        self.ninstr += 1

    def barrier(self):
        engs = ['pe', 'act', 'dve', 'pool', 'sp']
        toks = [(e, self.esem[e], self.ecnt[e]) for e in engs if self.ecnt[e]]
        for q in self.dsem:
            for i in range(NDS):
                if self.dcnt[q][i]:
                    toks.append((('d', q, i), self.dsem[q][i], self.dcnt[q][i]))
        for e in engs:
            for t in toks:
                if t[0] == e:
                    continue
                self._wait(e, t)
        self.lastw.clear()
        self.readers.clear()

    def finish(self, keys):
        self.barrier()


class Prog:
    def __init__(self, nlayers=DEPTH, stop=None, dbg=()):
        self.nlayers = nlayers
        self.stop = stop
        self.dbg = dbg
        nc = self.nc = bass.Bass("TRN2", target_bir_lowering=False)
        self.es = ExitStack()
        self.inputs = {}

    def din(self, name, shape, dt=F32):
        t = self.nc.dram_tensor(name, list(shape), dt, kind="ExternalInput").ap()
        self.inputs[name] = t
        return t

    def dout(self, name, shape, dt=F32):
        return self.nc.dram_tensor(name, list(shape), dt, kind="ExternalOutput").ap()

    def dscr(self, name, shape, dt):
        return self.nc.dram_tensor(name, list(shape), dt, kind="Internal").ap()

    def sb(self, name, shape, dt):
        return self.es.enter_context(self.nc.sbuf_tensor(name, list(shape), dt))

    def ps(self, name, shape, dt):
        return self.es.enter_context(self.nc.psum_tensor(name, list(shape), dt))

    def MM(self, out, lhsT, rhs, st, sp, R, W, **kw):
        self.kb.op('pe', R, W, lambda: self.nc.tensor.matmul(out, lhsT, rhs, start=st, stop=sp, **kw))

    def TR(self, out, in_, ident, R, W):
        self.kb.op('pe', R, W, lambda: self.nc.tensor.transpose(out, in_, ident))

    def ACT(self, out, in_, func, R, W, scale=1.0, bias=0.0):
        self.kb.op('act', R, W, lambda: self.nc.scalar.activation(out, in_, func, bias=bias, scale=scale))

    def TT(self, eng, out, in0, in1, op, R, W):
        e = self.nc.vector if eng == 'dve' else self.nc.gpsimd
        self.kb.op(eng, R, W, lambda: e.tensor_tensor(out, in0, in1, op))

    def TS(self, eng, out, in0, s1, op0, R, W, s2=None, op1=None, accum_out=None):
        e = self.nc.vector if eng == 'dve' else self.nc.gpsimd
        if op1 is None:
            self.kb.op(eng, R, W, lambda: e.tensor_scalar(out, in0, s1, s2, op0, accum_out=accum_out))
        else:
            self.kb.op(eng, R, W, lambda: e.tensor_scalar(out, in0, s1, s2, op0, op1, accum_out=accum_out))

    def STT(self, out, in0, scalar, in1, op0, op1, R, W):
        self.kb.op('dve', R, W, lambda: self.nc.vector.scalar_tensor_tensor(out, in0, scalar, in1, op0, op1))

    def CP(self, eng, out, in_, R, W):
        if eng == 'act':
            self.kb.op('act', R, W, lambda: self.nc.scalar.copy(out, in_))
        else:
            e = self.nc.vector if eng == 'dve' else self.nc.gpsimd
            self.kb.op(eng, R, W, lambda: e.tensor_copy(out, in_))

    def RCP(self, out, in_, R, W):
        self.kb.op('dve', R, W, lambda: self.nc.vector.reciprocal(out, in_))

    def MS(self, eng, ap, val, W):
        e = self.nc.vector if eng == 'dve' else self.nc.gpsimd
        self.kb.op(eng, [], W, lambda: e.memset(ap, val))

    def DMA(self, q, out, in_, R, W, **kw):
        self.kb.dma(q, out, in_, R, W, **kw)

    def build(self):
        nc = self.nc
        with self.es:
            self.kb = KB(nc, self.es)
            self.declare()
            self.setup_consts()
            self.phase_load_x()
            done = True
            for l in range(self.nlayers):
                self.layer(l)
                if self.stop is not None and self.stop[0] == l:
                    done = False
                    break
            if done:
                self.phase_final()
            self.debug_dump()
            self.kb.finish([])
        return nc

    def declare(self):
        L = DEPTH
        self.x = self.din("x", [TL, D]); self.ctx = self.din("ctx", [TC, D])
        self.cc = self.din("cc", [128, 16])
        self.ada_w = self.din("ada_w", [L, D, 6 * D]); self.ada_b = self.din("ada_b", [L, 128, 48])
        self.n1g = self.din("n1g", [L, 128, 8]); self.n2g = self.din("n2g", [L, 128, 8]); self.fng = self.din("fng", [128, 8])
        self.hgn = self.din("hgn", [L, 128, 8]); self.convw = self.din("convw", [L, 128, 24])
        self.qng = self.din("qng", [L, 128, 1]); self.kng = self.din("kng", [L, 128, 1])
        self.lbl = self.din("lbl", [128, 32])
        self.w_in = self.din("w_in", [L, D, 12800]); self.wk_dup = self.din("wk_dup", [L, D, 512])
        self.wpa = self.din("w_proj_a", [L, D, D]); self.wpb = self.din("w_proj_b", [L, D, D])
        self.wpc = self.din("w_proj_c", [L, D, D]); self.wout = self.din("w_out", [L, D, D])
        self.rw = self.din("router_w", [L, D, NE])
        self.wg = self.din("w_gate", [L, NE, D, D]); self.wu = self.din("w_up", [L, NE, D, D]); self.wd = self.din("w_down", [L, NE, D, D])
        self.c_ident = self.din("c_ident", [128, 128]); self.c_blk64 = self.din("c_blk64", [128, 128]); self.c_swap = self.din("c_swap", [128, 128])
        self.c_maskF = self.din("c_maskF", [128, 32]); self.c_maskB = self.din("c_maskB", [128, 32]); self.c_rowm = self.din("c_rowm", [128, 4])
        self.c_mask01 = self.din("c_mask01", [128, 512]); self.c_iota = self.din("c_iota", [128, 512]); self.c_jcol = self.din("c_jcol", [128, 4])
        self.c_cos = self.din("c_cos", [128, TL]); self.c_sin = self.din("c_sin", [128, TL])
        self.out = self.dout("out", [TL, D])
        self.xT = self.dscr("xT", [128, KC, T], F32)
        self.oa = self.dscr("s_oa", [128, KC, T], BF16); self.ob = self.dscr("s_ob", [128, KC, T], BF16); self.oc = self.dscr("s_oc", [128, KC, T], BF16)
        self.ga = self.dscr("s_ga", [128, KC, T], BF16); self.gb = self.dscr("s_gb", [128, KC, T], BF16); self.gc = self.dscr("s_gc", [128, KC, T], BF16)
        self.ysl = self.dscr("s_ysl", [NE, 128, 4, D], BF16); self.ysc = self.dscr("s_ysc", [NE, 32, D], BF16)
        self.posm_d = self.dscr("s_posm", [NE, T], F32); self.wm_d = self.dscr("s_wm", [NE, T], F32)
        self.vtok = self.dscr("s_vtok", [T // 64, 64, D], BF16)
        self.ofd = self.dscr("s_ofd", [128, KC, T], F32); self.obd = self.dscr("s_obd", [128, KC, T], F32)

        self.dbg_outs = []

    def scope(self, psum='default'):
        prog = self

        class _S:
            def __enter__(s):
                s.es = ExitStack(); s.es.__enter__()
                prog.uid = getattr(prog, 'uid', 0) + 1
                u = prog.uid
                pt = lambda name, shape, dt: s.es.enter_context(prog.nc.psum_tensor("%s_u%d" % (name, u), shape, dt))
                prog.P01 = pt("P01", [128, 1024], F32); prog.P23 = pt("P23", [128, 1024], F32); prog.P45 = pt("P45", [128, 1024], F32)
                prog.P6 = pt("P6", [128, 512], F32)
                if psum == 'attn':
                    prog.P7 = pt("P7", [128, 512], F32); prog.PT = None
                else:
                    prog.PT = pt("PT", [128, 1024], BF16); prog.P7 = None
                prog.P0 = prog.P01[:, 0:512]; prog.P1 = prog.P01[:, 512:1024]; prog.P2 = prog.P23[:, 0:512]; prog.P3 = prog.P23[:, 512:1024]
                prog.P4 = prog.P45[:, 0:512]; prog.P5 = prog.P45[:, 512:1024]
                return s

            def sb(s, name, shape, dt):
                prog.uid = getattr(prog, 'uid', 0) + 1
                return s.es.enter_context(prog.nc.sbuf_tensor("%s_u%d" % (name, prog.uid), list(shape), dt))

            def __exit__(s, *a):
                if a[0] is None:
                    prog.kb.barrier()
                return s.es.__exit__(*a)
        return _S()

    def setup_consts(self):
        sb = self.sb
        self.ident = sb("ident", [128, 128], F32); self.identb = sb("identb", [128, 128], BF16)
        self.blk64 = sb("blk64", [128, 128], F32); self.swapP = sb("swapP", [128, 128], F32)
        self.onesb = sb("onesb", [128, 512], BF16); self.onesf = sb("onesf", [128, 128], F32); self.zerob = sb("zerob", [128, 128], BF16)
        self.maskF = sb("maskF", [128, 32], F32); self.maskB = sb("maskB", [128, 32], F32); self.rowm = sb("rowm", [128, 4], F32)
        self.mask01 = sb("mask01", [128, 512], F32); self.iota = sb("iota", [128, 512], F32); self.jcol = sb("jcol", [128, 4], F32)
        for nm, dst, src in (('ident', self.ident, self.c_ident), ('blk64', self.blk64, self.c_blk64), ('swapP', self.swapP, self.c_swap),
                             ('maskF', self.maskF, self.c_maskF), ('maskB', self.maskB, self.c_maskB), ('rowm', self.rowm, self.c_rowm), ('mask01', self.mask01, self.c_mask01),
                             ('iota', self.iota, self.c_iota), ('jcol', self.jcol, self.c_jcol)):
            self.DMA('sp', dst[:], src, [], [nm])
        self.CP('dve', self.identb[:], self.ident[:], ['ident'], ['identb'])
        self.MS('dve', self.onesb[:], 1.0, ['onesb']); self.MS('dve', self.onesf[:], 1.0, ['onesf']); self.MS('dve', self.zerob[:], 0.0, ['zerob'])
        self.scc = sb("scc", [128, 16], F32)
        self.DMA('sp', self.scc[:], self.cc, [], ['scc'])
        self.ACT(self.scc[:], self.scc[:], AF.Silu, ['scc'], ['scc'])
        self.lb = sb("lb", [128, 32], F32)
        self.DMA('sp', self.lb[:], self.lbl, [], ['lb'])
        self.TT('dve', self.lb[:, 16:32], self.lb[:, 16:32], self.lb[:, 0:16], ALU.subtract, ['lb'], ['lb'])
        self.ACT(self.lb[:, 16:32], self.lb[:, 16:32], AF.Sigmoid, ['lb'], ['lb'])
        self.MS('dve', self.lb[:, 0:16], 0.0, ['lb'])
        self.oml = sb("oml", [128, 32], F32)
        self.ACT(self.oml[:], self.lb[:], AF.Identity, ['lb'], ['oml'], scale=-1.0, bias=1.0)
        self.fngs = sb("fngs", [128, 8], F32)
        self.DMA('sp', self.fngs[:], self.fng, [], ['fngs'])
        self.modv = sb("modv", [128, 48, 2], F32); self.G1 = sb("G1", [128, 8, 2], F32); self.G2 = sb("G2", [128, 8, 2], F32)
        self.vecs = sb("vecs", [128, 64], F32); self.vec2 = sb("vec2", [128, 64], F32)
        self.ptok = sb("ptok", [128, NT, NE], F32)
        self.kb.barrier()

    def wload(self, dst, src2d, key, c0=0, c1=None):
        v = src2d.rearrange("(k p) n -> p k n", p=128)
        if c1 is not None:
            v = v[:, :, c0:c1]
        n = v.shape[2]
        for k0 in range(0, 8, 2):
            self.DMA('pool', dst[:, k0:k0 + 2, :n], v[:, k0:k0 + 2, :], [], [key])

    def phase_load_x(self):
        with self.scope() as S:
            xbs = [S.sb("lx_xb%d" % i, [128, KC, 512], F32) for i in range(2)]
            stg = [S.sb("lx_st%d" % i, [128, 512], F32) for i in range(4)]
            for bi, (t0, W) in enumerate(BLOCKS):
                xb = xbs[bi % 2]; kx = 'xb%d' % (bi % 2)
                for j in range(W // 128):
                    tok = t0 + j * 128
                    src = self.ctx[tok:tok + 128, :] if tok < TC else self.x[tok - TC:tok - TC + 128, :]
                    for hf in range(2):
                        si = (j % 2) * 2 + hf
                        st = stg[si]; ks = 'st%d' % si
                        self.DMA('sp', st[:], src[:, hf * 512:(hf + 1) * 512], [], [ks])
                        pb = self.P0 if hf == 0 else self.P1; kp = 'P0' if hf == 0 else 'P1'
                        for c4 in range(4):
                            self.TR(pb[:, c4 * 128:(c4 + 1) * 128], st[:, c4 * 128:(c4 + 1) * 128], self.ident[:], [ks, 'ident'], [kp])
                        self.CP('act' if hf else 'dve', xb[:, hf * 4:(hf + 1) * 4, j * 128:(j + 1) * 128],
                                pb.rearrange("p (c t) -> p c t", c=4), [kp], [kx])
                self.DMA('sp', self.xT[:, :, t0:t0 + W], xb[:, :, :W], [kx], [('xT', bi)])

    def layer(self, l):
        last = (l == DEPTH - 1)
        self.phase_mod(l)
        hs = ExitStack(); hs.__enter__()
        self.hT = hs.enter_context(self.nc.sbuf_tensor("hT%d" % l, [128, KC, T], BF16))
        self.phase_norm1(l)
        if self.stop == (l, 'n1'):
            self.debug_dump(); self.kb.barrier(); hs.close(); return
        self.phase_attn(l)
        if self.stop == (l, 'attn'):
            self.kb.barrier(); hs.close(); return
        self.phase_conv(l)
        if self.stop == (l, 'conv'):
            self.kb.barrier(); hs.close(); return
        self.phase_hgrn(l)
        if self.stop == (l, 'hgrn'):
            self.kb.barrier(); hs.close(); return
        self.phase_gates(l)
        self.kb.barrier(); hs.close()
        self.phase_merge(l)
        if self.stop == (l, 'merge'):
            return
        hs = ExitStack(); hs.__enter__()
        self.h2t = hs.enter_context(self.nc.sbuf_tensor("h2t%d" % l, [128, NT, D], BF16))
        self.phase_router(l)
        self.phase_moeA(l)
        self.kb.barrier(); hs.close()
        self.phase_moeB(l)

    def groups(self, l):
        g = [dict(name='l', tok0=TC, ntok=TL, cap=512, j=0)]
        if l < DEPTH - 1:
            g.append(dict(name='c', tok0=0, ntok=TC, cap=32, j=1))
        return g

    def phase_mod(self, l):
        vs = self.vecs
        with self.scope() as S:
            wts = [S.sb("adaw%d" % i, [128, 8, 768], F32) for i in range(2)]
            self.DMA('sp', vs[:, 0:48], self.ada_b[l], [], ['vecs'])
            self.DMA('sp', vs[:, 48:56], self.n1g[l], [], ['vecs'])
            self.DMA('sp', vs[:, 56:64], self.n2g[l], [], ['vecs'])
            self.DMA('sp', self.vec2[:, 0:8], self.hgn[l], [], ['vec2'])
            self.DMA('sp', self.vec2[:, 8:32], self.convw[l], [], ['vec2'])
            self.DMA('sp', self.vec2[:, 32:33], self.qng[l], [], ['vec2'])
            self.DMA('sp', self.vec2[:, 33:34], self.kng[l], [], ['vec2'])
            for grp in range(8):
                wt = wts[grp % 2]; kw = 'adaw%d' % (grp % 2)
                self.DMA('sp', wt[:], self.ada_w[l].rearrange("(k p) n -> p k n", p=128)[:, :, grp * 768:(grp + 1) * 768], [], [kw])
                for m in range(6):
                    mc = grp * 6 + m
                    for kc in range(8):
                        self.MM(self.P4[:, mc * 2:mc * 2 + 2], wt[:, kc, m * 128:(m + 1) * 128], self.scc[:, kc::8], kc == 0, kc == 7, [kw, 'scc'], ['P4'])
            pv = self.P4[:, 0:96].rearrange("p (m j) -> p m j", j=2)
            for j in range(2):
                self.TT('dve', self.modv[:, :, j], pv[:, :, j], vs[:, 0:48], ALU.add, ['P4', 'vecs'], ['modv'])
            for j in range(2):
                self.TS('dve', self.G1[:, :, j], self.modv[:, 8:16, j], 1.0, ALU.add, ['modv'], ['G1'])
                self.TT('dve', self.G1[:, :, j], self.G1[:, :, j], vs[:, 48:56], ALU.mult, ['G1', 'vecs'], ['G1'])
                self.TS('dve', self.G2[:, :, j], self.modv[:, 32:40, j], 1.0, ALU.add, ['modv'], ['G2'])
                self.TT('dve', self.G2[:, :, j], self.G2[:, :, j], vs[:, 56:64], ALU.mult, ['G2', 'vecs'], ['G2'])

    def rstd_from_ps(self, ps, kps, rs, krs, W, nfeat):
        self.ACT(rs, ps, AF.Sqrt, [kps], [krs], scale=1.0 / nfeat, bias=EPS)
        self.RCP(rs, rs, [krs], [krs])

    def norm_block(self, S_, xb, kx, W, bi, sqs, rss, nfeat=D):
        pss = self.P5 if bi % 2 == 0 else self.P6; kps = 'P5' if bi % 2 == 0 else 'P6'
        rs = rss[bi % 2]; krs = 'rs%d' % (bi % 2)
        for c in range(KC):
            sqc = sqs[c % 2]; ksqc = 'sq%d' % (c % 2)
            self.ACT(sqc[:, :W], xb[:, c, :W], AF.Square, [kx], [ksqc])
            self.MM(pss[:, :W], self.onesb[:, 0:128], sqc[:, :W], c == 0, c == KC - 1, [ksqc, 'onesb'], [kps])
        self.rstd_from_ps(pss[:, :W], kps, rs[:, :W], krs, W, nfeat)
        return rs, krs

    def phase_norm1(self, l):
        with self.scope() as S:
            xbs = [S.sb("n1_xb%d" % i, [128, KC, 512], F32) for i in range(2)]
            sqs = [S.sb("n1_sq%d" % i, [128, 512], BF16) for i in range(2)]
            rss = [S.sb("n1_rs%d" % i, [128, 512], F32) for i in range(2)]
            tfs = [S.sb("n1_tf%d" % i, [128, 512], F32) for i in range(2)]
            for bi, (t0, W) in enumerate(BLOCKS):
                j = 1 if bi == 0 else 0
                xb = xbs[bi % 2]; kx = 'xb%d' % (bi % 2)
                self.DMA('sp', xb[:, :, :W], self.xT[:, :, t0:t0 + W], [('xT', bi)], [kx])
                rs, krs = self.norm_block(S, xb, kx, W, bi, sqs, rss)
                for c in range(KC):
                    tf = tfs[c % 2]; ktf = 'tf%d' % (c % 2)
                    self.TT('dve', tf[:, :W], xb[:, c, :W], rs[:, :W], ALU.mult, [kx, krs], [ktf])
                    self.ACT(self.hT[:, c, t0:t0 + W], tf[:, :W], AF.Identity, [ktf, 'G1', 'modv'], [('hT', bi)],
                             scale=self.G1[:, c, j:j + 1], bias=self.modv[:, c, j:j + 1])

    def proj(self, ps, kps, w, kw, c0, bi, W, t0):
        for kc in range(KC):
            self.MM(ps[:, :W], w[:, kc, c0:c0 + 128], self.hT[:, kc, t0:t0 + W], kc == 0, kc == KC - 1, [kw, ('hT', bi)], [kps])

    def qknorm_rope(self, ps, kps, gcol, W, lat, cs, kcs, tf, out_ap, kout, tb=None):
        (pS, kS), (pW, kW) = tb if tb is not None else ((self.P5, 'P5'), (self.P6, 'P6'))
        self.ACT(tf[0][:, :W], ps[:, :W], AF.Square, [kps], ['qt0'])
        self.MM(pS[:, :W], self.blk64[:], tf[0][:, :W], True, True, ['qt0', 'blk64'], [kS])
        self.rstd_from_ps(pS[:, :W], kS, tf[1][:, :W], 'qt1', W, 64)
        self.STT(tf[2][:, :W], ps[:, :W], gcol, tf[1][:, :W], ALU.mult, ALU.mult, [kps, 'qt1', 'vec2'], ['qt2'])
        if lat:
            self.MM(pW[:, :W], self.swapP[:], tf[2][:, :W], True, True, ['qt2', 'swapP'], [kW])
            self.TT('pool', tf[3][:, :W], tf[2][:, :W], cs[0][:, :W], ALU.mult, ['qt2', kcs], ['qt3'])
            self.TT('dve', tf[0][:, :W], pW[:, :W], cs[1][:, :W], ALU.mult, [kW, kcs], ['qt0'])
            self.TT('dve', out_ap, tf[3][:, :W], tf[0][:, :W], ALU.add, ['qt3', 'qt0'], [kout])
        else:
            self.CP('act', out_ap, tf[2][:, :W], ['qt2'], [kout])

    def phase_attn(self, l):
        last = (l == DEPTH - 1)
        with self.scope('attn') as S:
            wk = S.sb("at_wk", [128, KC, 512], BF16); wv = S.sb("at_wv", [128, KC, 256], BF16); wq = S.sb("at_wq", [128, KC, 1024], BF16)
            self.wload(wk, self.wk_dup[l], 'wk'); self.wload(wv, self.w_in[l], 'wv', O_AV, O_AV + 256); self.wload(wq, self.w_in[l], 'wq', O_AQ, O_AQ + 1024)
            KT = S.sb("at_KT", [128, T], BF16); VA = S.sb("at_VA", [128, NT, 128], BF16)
            css = [[S.sb("at_cs%d%d" % (i, k), [128, 512], F32) for k in range(2)] for i in range(2)]
            tfs = [[S.sb("at_tf%d_%d" % (s_, i), [128, 512], F32) for i in range(4)] for s_ in range(2)]
            QN = [S.sb("at_QN%d" % i, [128, T], BF16) for i in range(2)]
            pts = [S.sb("at_pt%d" % i, [128, 1024], BF16) for i in range(3)]
            oacp = [[S.sb("at_oc%d%d" % (i, h), [128, 512], F32) for h in range(2)] for i in range(2)]
            rc = S.sb("at_rc", [64, 512], F32)
            ost = [S.sb("at_os%d" % i, [128, 512], BF16) for i in range(2)]
            self.MS('dve', VA[:, :, 64:128], 1.0, ['VA'])
            st = dict(ncs=0, item=0)
            SPs = ((self.P01, ['P0', 'P1']), (self.P23, ['P2', 'P3']), (self.P45, ['P4', 'P5']))
            OAs = ((self.P6, 'P6'), (self.P7, 'P7'))

            psets = (((self.P0, 'P0'), (self.P1, 'P1'), (self.P2, 'P2')), ((self.P3, 'P3'), (self.P4, 'P4'), (self.P5, 'P5')))

            def prep_gen(w, kw, c0, gcol, bi, out_ap, kout, s_):
                t0, W = BLOCKS[bi]
                lat = bi > 0
                tf = tfs[s_]; q = lambda n_: 'qt%d_%d' % (n_, s_)
                (pp, kpp), (pS, kS), (pW, kW) = psets[s_]
                cs = css[s_]; kcs = 'cs%d' % s_
                if lat:
                    self.DMA('sp', cs[0][:], self.c_cos[:, t0 - TC:t0 - TC + 512], [], [kcs])
                    self.DMA('sp', cs[1][:], self.c_sin[:, t0 - TC:t0 - TC + 512], [], [kcs])
                self.proj(pp, kpp, w, kw, c0, bi, W, t0)
                self.ACT(tf[0][:, :W], pp[:, :W], AF.Square, [kpp], [q(0)])
                self.MM(pS[:, :W], self.blk64[:], tf[0][:, :W], True, True, [q(0), 'blk64'], [kS])
                yield
                self.ACT(tf[1][:, :W], pS[:, :W], AF.Ln, [kS], [q(1)], scale=1.0 / 64, bias=EPS)
                self.ACT(tf[1][:, :W], tf[1][:, :W], AF.Exp, [q(1)], [q(1)], scale=-0.5)
                yield
                self.STT(tf[2][:, :W], pp[:, :W], gcol, tf[1][:, :W], ALU.mult, ALU.mult, [kpp, q(1), 'vec2'], [q(2)])
                yield
                if lat:
                    self.MM(pW[:, :W], self.swapP[:], tf[2][:, :W], True, True, [q(2), 'swapP'], [kW])
                    self.TT('pool', tf[3][:, :W], tf[2][:, :W], cs[0][:, :W], ALU.mult, [q(2), kcs], [q(3)])
                    self.TT('dve', tf[0][:, :W], pW[:, :W], cs[1][:, :W], ALU.mult, [kW, kcs], [q(0)])
                    self.TT('dve', out_ap, tf[3][:, :W], tf[0][:, :W], ALU.add, [q(3), q(0)], [kout])
                else:
                    self.CP('act', out_ap, tf[2][:, :W], [q(2)], [kout])
                yield

            def run_lock(gens):
                gens = list(gens)
                while gens:
                    for g_ in list(gens):
                        try:
                            next(g_)
                        except StopIteration:
                            gens.remove(g_)

            def prep_many(jobs):
                for j0 in range(0, len(jobs), 2):
                    run_lock([prep_gen(*job, s_) for s_, job in enumerate(jobs[j0:j0 + 2])])

            for g in range(4):
                prep_many([(wk, 'wk', g * 128, self.vec2[:, 33:34], bi, KT[:, BLOCKS[bi][0]:BLOCKS[bi][0] + BLOCKS[bi][1]], ('KT', bi))
                           for bi in range(len(BLOCKS))])
                for tb_ in range(0, NT, 8):
                    n = min(8, NT - tb_)
                    for j in range(n):
                        tt = tb_ + j
                        for kc in range(KC):
                            self.MM(self.P3[:, j * 64:(j + 1) * 64], self.hT[:, kc, tt * 128:(tt + 1) * 128], wv[:, kc, g * 64:(g + 1) * 64],
                                    kc == 0, kc == KC - 1, ['wv', ('hT', self.blk_of(tt))], ['P3'])
                    self.CP('act', VA[:, tb_:tb_ + n, 0:64], self.P3[:, 0:n * 64].rearrange("p (j e) -> p j e", e=64), ['P3'], ['VA'])
                for qc in (2 * g, 2 * g + 1):
                    qn_all = QN[qc % 2]
                    blks = [bi for bi in range(len(BLOCKS)) if not (bi == 0 and last)]
                    prep_many([(wq, 'wq', qc * 128, self.vec2[:, 32:33], bi, qn_all[:, BLOCKS[bi][0]:BLOCKS[bi][0] + BLOCKS[bi][1]], ('qn', qc % 2, bi))
                               for bi in blks])
                    for _ in range(14):
                        self.MM(self.P6[:, :], self.identb[:], self.onesb[:, :], True, True, ['identb', 'onesb'], ['P6'])
                    for bi in blks:
                        t0, W = BLOCKS[bi]
                        lat = bi > 0
                        kqn = ('qn', qc % 2, bi)
                        kts = list(range(NT)) if lat else [0, 1]
                        it = st['item']; st['item'] += 1
                        osb = ost[it % 2]; kos = 'os%d' % (it % 2)

                        def issue_S(i):
                            kt = kts[i]; SP, ksp = SPs[i % 3]
                            for hf in range(2):
                                o = hf * 64
                                self.MM(SP[:, hf * 512:hf * 512 + W], KT[o:o + 64, kt * 128:(kt + 1) * 128], qn_all[o:o + 64, t0:t0 + W], True, True,
                                        [('KT', self.blk_of(kt)), kqn], [ksp[hf]])

                        issue_S(0)
                        if len(kts) > 1:
                            issue_S(1)
                        for i, kt in enumerate(kts):
                            SP, ksp = SPs[i % 3]
                            pt = pts[i % 3]; kpt = 'pt%d' % (i % 3)
                            self.ACT(pt[:, :].rearrange("p (n w) -> p n w", w=512)[:, :, :W],
                                     SP[:, :].rearrange("p (n w) -> p n w", w=512)[:, :, :W], AF.Exp, ksp, [kpt], scale=0.125)
                            if i + 2 < len(kts):
                                issue_S(i + 2)
                            for hf in range(2):
                                OA, koa = OAs[hf]
                                self.MM(OA[:, :W], VA[:, kt, :], pt[:, hf * 512:hf * 512 + W], i == 0, i == len(kts) - 1, ['VA', kpt], [koa])
                        for hf in range(2):
                            OA, koa = OAs[hf]
                            self.CP('dve', oacp[it % 2][hf][:, :W], OA[:, :W], [koa], [('oacp', it % 2, hf)])
                        for hf in range(2):
                            o = hf * 64
                            oc_ = oacp[it % 2][hf]; koc = ('oacp', it % 2, hf)
                            self.RCP(rc[:, :W], oc_[64:128, :W], [koc], ['rc'])
                            self.TT('dve', osb[o:o + 64, :W], oc_[0:64, :W], rc[:, :W], ALU.mult, [koc, 'rc'], [kos])
                        self.DMA('sp', self.oc[:, qc, t0:t0 + W], osb[:, :W], [kos], [('oc', qc, bi)])

    def blk_of(self, tt):
        tok = tt * 128
        return 0 if tok < TC else 1 + (tok - TC) // 512

    def phase_conv(self, l):
        last = (l == DEPTH - 1)
        with self.scope() as S:
            wB = S.sb("cv_wB", [128, KC, 1024], BF16); wC = S.sb("cv_wC", [128, KC, 1024], BF16); wX = S.sb("cv_wX", [128, KC, 1024], BF16)
            self.wload(wB, self.w_in[l], 'wB', O_CB, O_CB + 1024); self.wload(wC, self.w_in[l], 'wC', O_CC, O_CC + 1024)
            self.wload(wX, self.w_in[l], 'wX', O_CX, O_CX + 1024)
            Uc = S.sb("cv_Uc", [128, TC + 2], F32); Ul = S.sb("cv_Ul", [128, TL + 2], F32)
            dg = S.sb("cv_dg", [128, 3, 128], F32)
            tf = [S.sb("cv_tf%d" % i, [128, 512], F32) for i in range(2)]
            ost = [S.sb("cv_os%d" % i, [128, 512], BF16) for i in range(2)]
            for U, n in ((Uc, TC), (Ul, TL)):
                self.MS('dve', U[:, 0:1], 0.0, ['U']); self.MS('dve', U[:, n + 1:n + 2], 0.0, ['U'])
            for cc in range(8):
                for k in range(3):
                    self.TS('dve', dg[:, k, :], self.ident[:], self.vec2[:, 8 + k * 8 + cc:9 + k * 8 + cc], ALU.mult, ['ident', 'vec2'], ['dg'])
                for bi, (t0, W) in enumerate(BLOCKS):
                    if bi == 0 and last:
                        continue
                    U, tl = (Uc, t0) if bi == 0 else (Ul, t0 - TC)
                    self.proj(self.P4, 'P4', wC, 'wC', cc * 128, bi, W, t0)
                    self.proj(self.P5, 'P5', wX, 'wX', cc * 128, bi, W, t0)
                    self.CP('act', tf[0][:, :W], self.P5[:, :W], ['P5'], ['ctf0'])
                    self.TT('dve', U[:, 1 + tl:1 + tl + W], self.P4[:, :W], tf[0][:, :W], ALU.mult, ['P4', 'ctf0'], [('U', bi)])
                for bi, (t0, W) in enumerate(BLOCKS):
                    if bi == 0 and last:
                        continue
                    U, tl = (Uc, t0) if bi == 0 else (Ul, t0 - TC)
                    rk = [('U', b) for b in range(9)] + ['U', 'dg']
                    for k in range(3):
                        self.MM(self.P6[:, :W], dg[:, k, :], U[:, tl + k:tl + k + W], k == 0, k == 2, rk, ['P6'])
                    self.proj(self.P4, 'P4', wB, 'wB', cc * 128, bi, W, t0)
                    self.CP('act', tf[1][:, :W], self.P6[:, :W], ['P6'], ['ctf1'])
                    osb = ost[bi % 2]; kos = 'cos%d' % (bi % 2)
                    self.TT('dve', osb[:, :W], self.P4[:, :W], tf[1][:, :W], ALU.mult, ['P4', 'ctf1'], [kos])
                    self.DMA('sp', self.ob[:, cc, t0:t0 + W], osb[:, :W], [kos], [('ob', cc, bi)])

    def phase_hgrn(self, l):
        last = (l == DEPTH - 1)
        with self.scope() as S:
            wi = S.sb("hg_wi", [128, KC, 1024], BF16)
            self.wload(wi, self.w_in[l], 'hwi', O_HI, O_HI + 1024)
            vst = [S.sb("hg_vst%d" % i, [64, 1024], BF16) for i in range(2)]
            for tk in range(T // 64):
                st = vst[tk % 2]; kst = 'vst%d' % (tk % 2)
                bi = self.blk_of(tk // 2)
                for hf in range(2):
                    ps, kps = ((self.P4, 'P4'), (self.P5, 'P5'))[hf]
                    for kc in range(KC):
                        self.MM(ps[0:64, :], self.hT[:, kc, tk * 64:(tk + 1) * 64], wi[:, kc, hf * 512:(hf + 1) * 512], kc == 0, kc == KC - 1,
                                ['hwi', ('hT', bi)], [kps])
                    self.CP('act' if hf else 'dve', st[:, hf * 512:(hf + 1) * 512], ps[0:64, :], [kps], [kst])
                self.DMA('sp', self.vtok[tk], st[:], [kst], [('vtok', tk)])
        NCH = 4
        ALLPA = [('PA', c_) for c_ in range(NCH)]; ALLPU = [('PU', c_) for c_ in range(NCH)]
        with self.scope() as S:
            wts = [S.sb("hg_w%d" % f, [128, KC, 256], BF16) for f in range(3)]
            AMall = S.sb("hg_AMall", [128, NCH, 4, 32], BF16)
            ch = []
            for c in range(NCH):
                ch.append(dict(
                    tf=[S.sb("hg_tf%d_%d" % (c, i), [128, 512], F32) for i in range(4)],
                    q32=S.sb("hg_q32_%d" % c, [128, 512], F32),
                    QP=[S.sb("hg_QP%d_%d" % (c, i), [128, 512], BF16) for i in range(2)],
                    KP=[S.sb("hg_KP%d_%d" % (c, i), [128, 512], BF16) for i in range(2)],
                    KGT=[S.sb("hg_KGT%d_%d" % (c, i), [128, 4, 128], BF16) for i in range(2)],
                    DEC=[S.sb("hg_DEC%d_%d" % (c, i), [128, 16], F32) for i in range(2)],
                    KG=S.sb("hg_KG%d" % c, [128, 512], BF16), VH=S.sb("hg_VH%d" % c, [128, 4, 128], BF16),
                    VHm=S.sb("hg_VHm%d" % c, [128, 4, 4, 128], BF16), AM=AMall[:, c],
                    S32=S.sb("hg_S32_%d" % c, [128, 128], F32), Sb=S.sb("hg_Sb%d" % c, [128, 128], BF16),
                    ost=S.sb("hg_ost%d" % c, [128, 512], F32),
                    PO=(self.P0, self.P1, self.P2, self.P3)[c], kPO='P%d' % c,
                    PA=self.P4[:, c * CH:(c + 1) * CH], kPA=('PA', c), PU=self.P5[:, c * 128:(c + 1) * 128], kPU=('PU', c)))

            def mk(c, hd, d, w, kw, hloc):
                C = ch[c]; tf = C['tf']; K = lambda s: (s, c); K2 = lambda s, par: (s, c, par)
                lbc = self.lb[:, l * 16 + d * 8 + hd:l * 16 + d * 8 + hd + 1]
                omc = self.oml[:, l * 16 + d * 8 + hd:l * 16 + d * 8 + hd + 1]
                mask = self.maskF if d == 0 else self.maskB
                dst = self.ofd if d == 0 else self.obd

                def prep_gen(bi, par):
                    t0, W = BLOCKS[bi]
                    n = W // CH
                    QP = C['QP'][par]; KP = C['KP'][par]; KGT = C['KGT'][par]; DEC = C['DEC'][par]
                    self.proj(self.P6, 'P6', w[0], kw[0], hloc * 128, bi, W, t0)
                    self.CP('act', C['q32'][:, :W], self.P6[:, :W], ['P6'], [K('q32')])
                    yield
                    self.proj(self.P6, 'P6', w[1 + d], kw[1 + d], hloc * 128, bi, W, t0)
                    self.ACT(tf[0][:, :W], self.P6[:, :W], AF.Sigmoid, ['P6'], [K('t0')])
                    self.TS('dve', tf[0][:, :W], tf[0][:, :W], omc, ALU.mult, [K('t0'), 'oml', 'lb'], [K('t0')], s2=lbc, op1=ALU.add)
                    yield
                    self.ACT(tf[1][:, :W], tf[0][:, :W], AF.Ln, [K('t0')], [K('t1')])
                    self.ACT(tf[2][:, :W], tf[0][:, :W], AF.Identity, [K('t0')], [K('t2')], scale=-1.0, bias=1.0)
                    yield
                    Bt = tf[3]
                    if d == 0:
                        self.kb.op('dve', [K('t1'), 'mask01'], [K('t3')], lambda: self.nc.vector.tensor_tensor_scan(
                            Bt[:, :W], self.mask01[:, :W], tf[1][:, :W], 0.0, ALU.mult, ALU.add))
                    else:
                        self.kb.op('dve', [K('t1'), 'mask01'], [K('t3')], lambda: self.nc.vector.tensor_tensor_scan(
                            Bt[:, :W][:, ::-1], self.mask01[:, :W], tf[1][:, :W][:, ::-1], 0.0, ALU.mult, ALU.add))
                    Bv = Bt[:, :W].rearrange("p (n c) -> p n c", c=CH)
                    bl = Bv[:, :, CH - 1:CH] if d == 0 else Bv[:, :, 0:1]
                    yield
                    self.ACT(tf[0][:, :W], Bt[:, :W], AF.Exp, [K('t3')], [K('t0')])
                    self.ACT(tf[1][:, :W], Bt[:, :W], AF.Exp, [K('t3')], [K('t1')], scale=-1.0)
                    self.ACT(DEC[:, 0:n], bl.rearrange("p n c -> p (n c)"), AF.Exp, [K('t3')], [K2('DEC', par)])
                    self.TT('dve', QP[:, :W], C['q32'][:, :W], tf[0][:, :W], ALU.mult, [K('q32'), K('t0')], [K2('QP', par)])
                    self.TT('pool', KP[:, :W], tf[2][:, :W], tf[1][:, :W], ALU.mult, [K('t2'), K('t1')], [K2('KP', par)])
                    yield
                    self.TT('dve', tf[0][:, :W].rearrange("p (n c) -> p n c", c=CH), bl.broadcast_to([128, n, CH]), Bv, ALU.subtract, [K('t3')], [K('t0')])
                    self.ACT(tf[0][:, :W], tf[0][:, :W], AF.Exp, [K('t0')], [K('t0')])
                    self.TT('pool', C['KG'][:, :W], tf[2][:, :W], tf[0][:, :W], ALU.mult, [K('t2'), K('t0')], [K('KG')])
                    yield
                    for j in range(W // 128):
                        self.TR(self.PT[:, j * 128:(j + 1) * 128], C['KG'][:, j * 128:(j + 1) * 128], self.identb[:], [K('KG'), 'identb'], ['PT'])
                    self.CP('act', KGT[:, 0:W // 128, :], self.PT[:, 0:W].rearrange("p (j e) -> p j e", e=128), ['PT'], [K2('KGT', par)])
                    yield

                def step_gen(bi, par):
                    t0, W = BLOCKS[bi]
                    n = W // CH
                    QP = C['QP'][par]; KP = C['KP'][par]; KGT = C['KGT'][par]; DEC = C['DEC'][par]
                    kQP, kKP, kKGT, kDEC = K2('QP', par), K2('KP', par), K2('KGT', par), K2('DEC', par)
                    self.DMA('sp', C['VH'][:, 0:W // 128, :],
                             self.vtok[t0 // 64:(t0 + W) // 64, :, hd * 128:(hd + 1) * 128].rearrange("(j two) p e -> (two p) j e", two=2),
                             [('vtok', tk) for tk in range(t0 // 64, (t0 + W) // 64)], [K('VH')])
                    for q_ in range(4):
                        if q_ < 2:
                            self.ACT(C['VHm'][:, 0:W // 128, q_, :], C['VH'][:, 0:W // 128, :], AF.Identity, [K('VH'), 'rowm'], [(K('VHm'), q_)],
                                     scale=self.rowm[:, q_:q_ + 1])
                        else:
                            self.TS('dve', C['VHm'][:, 0:W // 128, q_, :], C['VH'][:, 0:W // 128, :], self.rowm[:, q_:q_ + 1], ALU.mult,
                                    [K('VH'), 'rowm'], [(K('VHm'), q_)])
                    yield
                    cis = list(range(n)) if d == 0 else list(range(n - 1, -1, -1))

                    def emitA(ci):
                        t = ci * CH; T0 = (t // 128) * 128
                        self.MM(C['PA'], KP[:, T0:T0 + 128], QP[:, t:t + CH], True, True, [kKP, kQP], [C['kPA']])

                    def emitM(ci):
                        if c >= 2:
                            return
                        t = ci * CH; q = (t % 128) // CH
                        rows = slice(q * CH, (q + 1) * CH)
                        self.TT('dve', AMall[rows, c:c + 3:2, q, :],
                                self.P4[rows, 0:NCH * CH].rearrange("p (c x) -> p c x", x=CH)[:, c:c + 3:2, :],
                                mask[rows, :][:, None, :].broadcast_to([CH, 2, CH]), ALU.mult,
                                ALLPA + ['maskF', 'maskB'], [(('AM', c), q), (('AM', c + 2), q)])

                    emitA(cis[0])
                    yield
                    emitM(cis[0])
                    yield
                    for idx, ci in enumerate(cis):
                        t = ci * CH; tt = t // 128; q = (t % 128) // CH
                        if idx + 1 < len(cis):
                            emitA(cis[idx + 1])
                        yield
                        if idx + 1 < len(cis):
                            emitM(cis[idx + 1])
                        yield
                        self.MM(C['PU'], KGT[:, tt, :], C['VHm'][:, tt, q, :], True, True, [kKGT, (K('VHm'), q)], [C['kPU']])
                        yield
                        self.MM(C['PO'][:, t:t + CH], C['VH'][:, tt, :], C['AM'][:, q, :], True, False, [K('VH'), (K('AM'), q)], [C['kPO']])
                        self.MM(C['PO'][:, t:t + CH], C['Sb'][:, :], QP[:, t:t + CH], False, True, [K('Sb'), kQP], [C['kPO']])
                        yield
                        self.STT(C['S32'][:, :], C['S32'][:, :], DEC[:, ci:ci + 1], C['PU'], ALU.mult, ALU.add, [K('S32'), kDEC] + ALLPU, [K('S32')])
                        yield
                        self.CP('act', C['Sb'][:, :], C['S32'][:, :], [K('S32')], [K('Sb')])
                        yield
                    if not (bi == 0 and last):
                        self.CP('act', C['ost'][:, :W], C['PO'][:, :W], [C['kPO']], [K('ost')])
                        self.DMA('sp', dst[:, hd, t0:t0 + W], C['ost'][:, :W], [K('ost')], [('ofb', d, hd, bi)])
                    yield

                return prep_gen, step_gen

            def advance(gens):
                for g_ in list(gens):
                    try:
                        next(g_)
                    except StopIteration:
                        gens.remove(g_)

            for hp in range(4):
                kw = ['hgw%d' % f for f in range(3)]
                for f, off in enumerate((O_HQ, O_HF, O_HB)):
                    self.wload(wts[f], self.w_in[l], kw[f], off + hp * 256, off + hp * 256 + 256)
                self.MS('dve', AMall[:], 0.0, [(('AM', c_), q_) for c_ in range(NCH) for q_ in range(4)])
                fns = []; orders = []
                for hl in range(2):
                    for d in range(2):
                        c = hl * 2 + d
                        self.MS('dve', ch[c]['S32'][:], 0.0, [('S32', c)]); self.MS('dve', ch[c]['Sb'][:], 0.0, [('Sb', c)])
                        fns.append(mk(c, hp * 2 + hl, d, wts, kw, hl))
                        orders.append(list(range(9)) if d == 0 else [0] + list(range(8, 0, -1)))
                nb = len(orders[0])
                gens = [fns[c][0](orders[c][0], 0) for c in range(NCH)]
                while gens:
                    advance(gens)
                for i in range(nb):
                    steps = [fns[c][1](orders[c][i], i % 2) for c in range(NCH)]
                    nxt = [fns[c][0](orders[c][i + 1], (i + 1) % 2) for c in range(NCH)] if i + 1 < nb else []
                    nsub = 4 + (BLOCKS[orders[0][i]][1] // CH) * 6
                    period = max(1, nsub // 9)
                    k = 0
                    while steps:
                        advance(steps)
                        k += 1
                        if nxt and k % period == 0:
                            advance(nxt)
                    while nxt:
                        advance(nxt)
        with self.scope() as S:
            wgt = S.sb("hg_wg", [128, KC, 1024], BF16)
            self.wload(wgt, self.w_in[l], 'hwg', O_HG, O_HG + 1024)
            G = 3
            ins = [[S.sb("hg_in%d%d" % (i, k), [128, 512], F32) for k in range(2)] for i in range(G)]
            tfr = [[S.sb("hg_rtf%d_%d" % (i, k), [128, 512], F32) for k in range(3)] for i in range(G)]
            sqbs = [S.sb("hg_sq%d" % i, [128, 512], BF16) for i in range(G)]; ost = [S.sb("hg_os%d" % i, [128, 512], BF16) for i in range(G)]
            ssb = ((self.P0, 'P0'), (self.P1, 'P1'), (self.P2, 'P2')); gbk = ((self.P3, 'P3'), (self.P4, 'P4'), (self.P5, 'P5'))

            def ro_gen(hd, bi, s_):
                t0, W = BLOCKS[bi]
                i2 = ins[s_]; ki = [('hin', s_, k) for k in range(2)]; tf = tfr[s_]; sqb = sqbs[s_]; osb = ost[s_]
                r = lambda n_: ('rr', n_, s_)
                (PS_, kPS), (PG, kPG) = ssb[s_], gbk[s_]
                self.DMA('sp', i2[0][:, :W], self.ofd[:, hd, t0:t0 + W], [], [ki[0]])
                self.DMA('sp', i2[1][:, :W], self.obd[:, hd, t0:t0 + W], [], [ki[1]])
                o = tf[0]
                self.TT('pool', o[:, :W], i2[0][:, :W], i2[1][:, :W], ALU.add, ki, [r(0)])
                self.ACT(sqb[:, :W], o[:, :W], AF.Square, [r(0)], [r(3)])
                self.MM(PS_[:, :W], self.onesb[:, 0:128], sqb[:, :W], True, True, [r(3), 'onesb'], [kPS])
                self.proj(PG, kPG, wgt, 'hwg', hd * 128, bi, W, t0)
                yield
                self.ACT(tf[1][:, :W], PS_[:, :W], AF.Ln, [kPS], [r(1)], scale=1.0 / 128, bias=EPS)
                self.ACT(tf[1][:, :W], tf[1][:, :W], AF.Exp, [r(1)], [r(1)], scale=-0.5)
                yield
                self.STT(tf[2][:, :W], o[:, :W], self.vec2[:, hd:hd + 1], tf[1][:, :W], ALU.mult, ALU.mult, [r(0), r(1), 'vec2'], [r(2)])
                yield
                self.ACT(tf[1][:, :W], PG[:, :W], AF.Silu, [kPG, r(1)], [r(1)])
                yield
                self.TT('dve', osb[:, :W], tf[2][:, :W], tf[1][:, :W], ALU.mult, [r(2), r(1)], [('ros', s_)])
                self.DMA('sp', self.oa[:, hd, t0:t0 + W], osb[:, :W], [('ros', s_)], [('oa', hd, bi)])
                yield

            items = [(hd, bi) for hd in range(8) for bi in range(len(BLOCKS)) if not (bi == 0 and last)]
            for g0 in range(0, len(items), G):
                gens = [ro_gen(hd, bi, s_) for s_, (hd, bi) in enumerate(items[g0:g0 + G])]
                while gens:
                    for g_ in list(gens):
                        try:
                            next(g_)
                        except StopIteration:
                            gens.remove(g_)

    def phase_gates(self, l):
        last = (l == DEPTH - 1)
        with self.scope() as S:
            ws = [S.sb("gt_w%d" % i, [128, KC, 1024], BF16) for i in range(3)]
            ost = [S.sb("gt_os%d" % i, [128, 512], BF16) for i in range(2)]
            for f, (off, dst) in enumerate(((O_GA, self.ga), (O_GB, self.gb), (O_GC, self.gc))):
                self.wload(ws[f], self.w_in[l], 'gw%d' % f, off, off + 1024)
                cnt = 0
                for oc in range(8):
                    for bi, (t0, W) in enumerate(BLOCKS):
                        if bi == 0 and last:
                            continue
                        ps, kps = ((self.P4, 'P4'), (self.P5, 'P5'))[cnt % 2]
                        osb = ost[cnt % 2]; kos = 'gos%d' % (cnt % 2); cnt += 1
                        self.proj(ps, kps, ws[f], 'gw%d' % f, oc * 128, bi, W, t0)
                        self.ACT(osb[:, :W], ps[:, :W], AF.Sigmoid, [kps], [kos])
                        self.DMA('sp', dst[:, oc, t0:t0 + W], osb[:, :W], [kos], [('g', f, oc, bi)])

    def phase_merge(self, l):
        last = (l == DEPTH - 1)
        with self.scope() as S:
            wa = S.sb("mg_wa", [128, KC, 1024], BF16); wb = S.sb("mg_wb", [128, KC, 1024], BF16)
            wc = S.sb("mg_wc", [128, KC, 1024], BF16); wo = S.sb("mg_wo", [128, KC, 1024], BF16)
            self.wload(wa, self.wpa[l], 'wa'); self.wload(wb, self.wpb[l], 'wb'); self.wload(wc, self.wpc[l], 'wc'); self.wload(wo, self.wout[l], 'wo')
            st = [[S.sb("mg_s%d%d" % (i, k), [128, KC, 256], BF16) for k in range(6)] for i in range(2)]
            xbs = [S.sb("mg_xb%d" % i, [128, KC, 256], F32) for i in range(2)]
            yb = [S.sb("mg_yb%d" % i, [128, KC, 256], BF16) for i in range(2)]
            tfm = [[S.sb("mg_tf%d_%d" % (s_, i), [128, 256], F32) for i in range(3)] for s_ in range(2)]
            srcs = (self.oa, self.ob, self.oc, self.ga, self.gb, self.gc)
            W = 256
            for bi, (t0, _) in enumerate(B256):
                if bi == 0 and last:
                    continue
                j = 1 if bi == 0 else 0
                s = st[bi % 2]; ks = ['ms%d%d' % (bi % 2, k) for k in range(6)]
                for k in range(6):
                    self.DMA('sp', s[k][:], srcs[k][:, :, t0:t0 + W], [], [ks[k]])
                xb = xbs[bi % 2]; kx = 'mxb%d' % (bi % 2)
                self.DMA('sp', xb[:], self.xT[:, :, t0:t0 + W], [], [kx])
                y = yb[bi % 2]; ky = 'myb%d' % (bi % 2)
                for oc in range(8):
                    so = oc % 2
                    tf = tfm[so]; kt = ['mtf%d_%d' % (k_, so) for k_ in range(3)]
                    banks = (((self.P0, 'P0'), (self.P1, 'P1'), (self.P2, 'P2')), ((self.P3, 'P3'), (self.P4, 'P4'), (self.P5, 'P5')))[so]
                    for k, (w, kw) in enumerate(((wa, 'wa'), (wb, 'wb'), (wc, 'wc'))):
                        ps, kps = banks[k]
                        for kc in range(KC):
                            self.MM(ps[:, :W], w[:, kc, oc * 128:(oc + 1) * 128], s[k][:, kc, :], kc == 0, kc == KC - 1, [kw, ks[k]], [kps])
                        self.TT('dve', tf[k][:, :], ps[:, :W], s[3 + k][:, oc, :], ALU.mult, [kps, ks[3 + k]], [kt[k]])
                    self.TT('pool', tf[0][:, :], tf[0][:, :], tf[1][:, :], ALU.add, [kt[0], kt[1]], [kt[0]])
                    self.TT('pool', y[:, oc, :], tf[0][:, :], tf[2][:, :], ALU.add, [kt[0], kt[2]], [(ky, oc)])
                for oc in range(8):
                    ps, kps = ((self.P6, 'P6'), (self.P0, 'P0'))[oc % 2]
                    for kc in range(KC):
                        self.MM(ps[:, :W], wo[:, kc, oc * 128:(oc + 1) * 128], y[:, kc, :], kc == 0, kc == KC - 1, ['wo', (ky, kc)], [kps])
                    self.STT(xb[:, oc, :], ps[:, :W], self.modv[:, 16 + oc, j:j + 1], xb[:, oc, :], ALU.mult, ALU.add, [kps, 'modv', kx], [kx])
                self.DMA('sp', self.xT[:, :, t0:t0 + W], xb[:], [kx], [('xTm', bi)])

    def phase_router(self, l):
        grps = self.groups(l)
        with self.scope() as S:
            xbs = [S.sb("rt_xb%d" % i, [128, KC, 512], F32) for i in range(2)]
            sqs = [S.sb("rt_sq%d" % i, [128, 512], BF16) for i in range(2)]
            rss = [S.sb("rt_rs%d" % i, [128, 512], F32) for i in range(2)]
            tfs = [S.sb("rt_tf%d" % i, [128, 512], F32) for i in range(2)]
            h2f = S.sb("rt_h2f", [128, KC, 512], F32); h2b = S.sb("rt_h2b", [128, KC, 512], BF16)
            rwt = S.sb("rt_rw", [128, KC, NE], F32)
            A = S.sb("rt_A", [NE, T], F32); Bm = S.sb("rt_B", [NE, T], F32); Cp = S.sb("rt_C", [NE, T], F32)
            sv = S.sb("rt_sv", [NE, 8], F32)
            self.DMA('sp', rwt[:], self.rw[l].rearrange("(k p) n -> p k n", p=128), [], ['rwt'])
            for bi, (t0, W) in enumerate(BLOCKS):
                if bi == 0 and len(grps) == 1:
                    continue
                j = 1 if bi == 0 else 0
                xb = xbs[bi % 2]; kx = 'xb%d' % (bi % 2)
                self.DMA('sp', xb[:, :, :W], self.xT[:, :, t0:t0 + W], [], [kx])
                rs, krs = self.norm_block(S, xb, kx, W, bi, sqs, rss)
                for c in range(KC):
                    tf = tfs[c % 2]; ktf = 'tf%d' % (c % 2)
                    self.TT('dve', tf[:, :W], xb[:, c, :W], rs[:, :W], ALU.mult, [kx, krs], [ktf])
                    self.ACT(h2f[:, c, :W], tf[:, :W], AF.Identity, [ktf, 'G2', 'modv'], ['h2f'],
                             scale=self.G2[:, c, j:j + 1], bias=self.modv[:, 24 + c, j:j + 1])
                self.CP('pool', h2b[:, :, :W], h2f[:, :, :W], ['h2f'], ['h2b'])
                for kc in range(KC):
                    self.MM(self.P4[0:NE, :W], rwt[:, kc, :], h2f[:, kc, :W], kc == 0, kc == KC - 1, ['rwt', 'h2f'], ['P4'])
                self.ACT(A[:, t0:t0 + W], self.P4[0:NE, :W], AF.Exp, ['P4'], [('A', bi)])
                self.MM(self.P0[0:NE, :W], self.onesf[0:NE, 0:NE], A[:, t0:t0 + W], True, True, [('A', bi), 'onesf'], ['P0'])
                self.RCP(Bm[:, t0:t0 + W], self.P0[0:NE, :W], ['P0'], [('B', bi)])
                self.TT('dve', A[:, t0:t0 + W], A[:, t0:t0 + W], Bm[:, t0:t0 + W], ALU.mult, [('A', bi), ('B', bi)], [('A', bi)])
                for jt in range(W // 128):
                    tt = t0 // 128 + jt
                    for kc in range(KC):
                        self.TR(self.PT[:, kc * 128:(kc + 1) * 128], h2b[:, kc, jt * 128:(jt + 1) * 128], self.identb[:], ['h2b', 'identb'], ['PT'])
                    self.CP('act' if jt % 2 else 'dve', self.h2t[:, tt, :], self.PT[:, :], ['PT'], [('h2t', tt)])
            allA = [('A', b) for b in range(9)]; allB = [('B', b) for b in range(9)]
            LO, HI, MID, CNT, M, DL, DH = (sv[:, i:i + 1] for i in range(7))
            for g in grps:
                a0, n, cap = g['tok0'], g['ntok'], g['cap']
                Ag = A[:, a0:a0 + n]; Bg = Bm[:, a0:a0 + n]; Cg = Cp[:, a0:a0 + n]
                self.MS('dve', LO, 0.0, ['sv']); self.MS('dve', HI, 1.0, ['sv'])
                for it in range(26):
                    self.TS('dve', MID, LO, HI, ALU.add, ['sv'], ['sv'], s2=0.5, op1=ALU.mult)
                    self.TS('dve', Bg, Ag, MID, ALU.is_gt, allA + ['sv'], allB + ['sv'], s2=None, op1=ALU.add, accum_out=CNT)
                    self.TS('dve', M, CNT, float(cap), ALU.is_ge, ['sv'], ['sv'])
                    self.TT('dve', DL, MID, LO, ALU.subtract, ['sv'], ['sv'])
                    self.TT('dve', DH, HI, MID, ALU.subtract, ['sv'], ['sv'])
                    self.STT(LO, DL, M, LO, ALU.mult, ALU.add, ['sv'], ['sv'])
                    self.STT(HI, DH, M, MID, ALU.mult, ALU.add, ['sv'], ['sv'])
                self.TS('dve', Bg, Ag, LO, ALU.is_gt, allA + ['sv'], allB)
                for c0 in range(0, n, 512):
                    w = min(512, n - c0)
                    init = 0.0 if c0 == 0 else Cg[:, c0 - 1:c0]
                    self.kb.op('dve', allB + ['onesf', 'C'], ['C'], lambda c0=c0, w=w, init=init: self.nc.vector.tensor_tensor_scan(
                        Cg[:, c0:c0 + w], self.onesf[0:NE, 0:1].broadcast_to([NE, w]), Bg[:, c0:c0 + w], init, ALU.mult, ALU.add))
                self.TT('dve', Cg, Cg, Bg, ALU.mult, ['C'] + allB, ['C'])
                self.TS('dve', Cg, Cg, -1.0, ALU.add, ['C'], ['C'])
                self.TT('dve', Ag, Ag, Bg, ALU.mult, allA + allB, allA)
                self.DMA('sp', self.posm_d[:, a0:a0 + n], Cg, ['C'], ['posm_d'])
                self.DMA('sp', self.wm_d[:, a0:a0 + n], Ag, allA, ['wm_d'])
                for tt in range(a0 // 128, (a0 + n) // 128):
                    self.TR(self.P4[:, (tt % 32) * NE:(tt % 32 + 1) * NE], Cp[:, tt * 128:(tt + 1) * 128], self.ident[0:NE, 0:NE], ['C', 'ident'], ['P4'])
                    if tt % 32 == 31 or tt == (a0 + n) // 128 - 1:
                        b0 = (tt // 32) * 32
                        self.CP('dve', self.ptok[:, b0:tt + 1, :], self.P4[:, 0:(tt + 1 - b0) * NE].rearrange("p (t e) -> p t e", e=NE), ['P4'], ['ptok'])

    def phase_moeA(self, l):
        grps = self.groups(l)
        with self.scope() as S:
            wring = [S.sb("ma_w%d" % i, [128, KC, 1024], BF16) for i in range(4)]
            SEL = S.sb("ma_sel", [128, 16, 512], BF16)
            XS = S.sb("ma_xs", [128, KC, 512], BF16); HID = S.sb("ma_hid", [128, KC, 512], BF16)
            YS = [S.sb("ma_ys%d" % i, [128, 4, 1024], BF16) for i in range(2)]
            sg = [S.sb("ma_sg%d" % i, [128, 512], F32) for i in range(2)]
            nsel = 0
            srcw = (self.wg, self.wu, self.wd)

            def slot(e, m):
                return (3 * e + m) % 4

            def load(e, m):
                if e < NE:
                    self.wload(wring[slot(e, m)], srcw[m][l, e], 'mw%d' % slot(e, m))

            for m in range(3):
                load(0, m)
            for e in range(NE):
                wr = [wring[slot(e, m)] for m in range(3)]
                kwr = ['mw%d' % slot(e, m) for m in range(3)]
                load(e + 1, 0)
                for g in grps:
                    cap = g['cap']; tts = list(range(g['tok0'] // 128, (g['tok0'] + g['ntok']) // 128))
                    for h0 in range(0, len(tts), 16):
                        part = tts[h0:h0 + 16]
                        for i, tt in enumerate(part):
                            self.TS('dve', SEL[:, i, :cap], self.iota[:, :cap], self.ptok[:, tt, e:e + 1], ALU.is_equal,
                                    ['iota', 'ptok'], [('sel', i)])
                        for kc in range(KC):
                            ps, kps = ((self.P4, 'P4'), (self.P5, 'P5'))[kc % 2]
                            for i, tt in enumerate(part):
                                self.MM(ps[:, :cap], self.h2t[:, tt, kc * 128:(kc + 1) * 128], SEL[:, i, :cap], i == 0, i == len(part) - 1,
                                        [('sel', i)], [kps])
                            if h0 == 0:
                                self.CP('act', XS[:, kc, :cap], ps[:, :cap], [kps], [('xs', kc)])
                            else:
                                self.TT('dve', XS[:, kc, :cap], ps[:, :cap], XS[:, kc, :cap], ALU.add, [kps, ('xs', kc)], [('xs', kc)])
                    xk = [('xs', kc) for kc in range(KC)]
                    for fc in range(8):
                        for kc in range(KC):
                            self.MM(self.P0[:, :cap], wr[0][:, kc, fc * 128:(fc + 1) * 128], XS[:, kc, :cap], kc == 0, kc == KC - 1, [kwr[0]] + xk, ['P0'])
                        for kc in range(KC):
                            self.MM(self.P1[:, :cap], wr[1][:, kc, fc * 128:(fc + 1) * 128], XS[:, kc, :cap], kc == 0, kc == KC - 1, [kwr[1]] + xk, ['P1'])
                        s_ = sg[fc % 2]; ksg = 'sg%d' % (fc % 2)
                        self.ACT(s_[:, :cap], self.P0[:, :cap], AF.Silu, ['P0'], [ksg])
                        self.TT('dve', HID[:, fc, :cap], self.P1[:, :cap], s_[:, :cap], ALU.mult, ['P1', ksg], [('hid', fc)])
                    hk = [('hid', fc) for fc in range(8)]
                    if g is grps[-1]:
                        load(e + 1, 1)
                    ys = YS[nsel % 2]; kys = 'ys%d' % (nsel % 2); nsel += 1
                    njt = (cap + 127) // 128
                    cnt = 0
                    for jt in range(njt):
                        nj = min(128, cap - jt * 128)
                        for db in range(2):
                            ps, kps = ((self.P2, 'P2'), (self.P3, 'P3'))[cnt % 2]; cnt += 1
                            for fc in range(8):
                                self.MM(ps[0:nj, :], HID[:, fc, jt * 128:jt * 128 + nj], wr[2][:, fc, db * 512:(db + 1) * 512], fc == 0, fc == 7, [kwr[2]] + hk, [kps])
                            self.CP('act' if db else 'dve', ys[0:nj, jt, db * 512:(db + 1) * 512], ps[0:nj, :], [kps], [kys])
                    if g is grps[-1]:
                        load(e + 1, 2)
                    if g['name'] == 'l':
                        self.DMA('sp', self.ysl[e], ys[:], [kys], [('ysl', e)])
                    else:
                        self.DMA('sp', self.ysc[e], ys[0:32, 0, :], [kys], [('ysc', e)])

    def phase_moeB(self, l):
        grps = self.groups(l)
        with self.scope() as S:
            YA = S.sb("mb_ya", [128, NE, 4, 1024], BF16)
            pb = [S.sb("mb_pb%d" % i, [128, 256], F32) for i in range(3)]; wbc = [S.sb("mb_wb%d" % i, [128, 256], F32) for i in range(3)]
            selt = [S.sb("mb_st%d" % i, [128, 4, 256], BF16) for i in range(3)]
            xbs = [S.sb("mb_xb%d" % i, [128, KC, 256], F32) for i in range(2)]
            W = 256
            acc = [(self.P0, 'P0'), (self.P1, 'P1'), (self.P2, 'P2'), (self.P3, 'P3')]
            for g in grps:
                cap = g['cap']; j = g['j']
                njt = (cap + 127) // 128
                if g['name'] == 'l':
                    for e in range(NE):
                        self.DMA('sp', YA[:, e], self.ysl[e], [], ['YA'])
                else:
                    for e in range(NE):
                        self.DMA('sp', YA[0:32, e, 0, :], self.ysc[e], [], ['YA'])
                cnt = 0
                for t0 in range(g['tok0'], g['tok0'] + g['ntok'], W):
                    bi = t0 // W
                    xb = xbs[bi % 2]; kx = 'bxb%d' % (bi % 2)
                    self.DMA('sp', xb[:], self.xT[:, :, t0:t0 + W], [], [kx])
                    for (ps, kps) in acc:
                        self.MM(ps[:, :], self.zerob[:, :], self.onesb[:, :], True, False, ['zerob', 'onesb'], [kps], skip_group_check=True)
                    for e in range(NE):
                        p_ = pb[cnt % 3]; w_ = wbc[cnt % 3]; s_ = selt[cnt % 3]
                        kp_, kw_, ks_ = 'pb%d' % (cnt % 3), 'wb%d' % (cnt % 3), 'st%d' % (cnt % 3); cnt += 1
                        self.DMA('sp', p_[:], self.posm_d[e:e + 1, t0:t0 + W].broadcast_to([128, W]), [], [kp_])
                        self.DMA('sp', w_[:], self.wm_d[e:e + 1, t0:t0 + W].broadcast_to([128, W]), [], [kw_])
                        for jt in range(njt):
                            nj = min(128, cap - jt * 128)
                            self.STT(s_[0:nj, jt, :], p_[0:nj, :], self.jcol[0:nj, jt:jt + 1], w_[0:nj, :], ALU.is_equal, ALU.mult, [kp_, kw_, 'jcol'], [ks_])
                        for c in range(KC):
                            ps, kps = acc[c // 2]
                            for jt in range(njt):
                                nj = min(128, cap - jt * 128)
                                lastmm = (e == NE - 1 and jt == njt - 1)
                                self.MM(ps[:, (c % 2) * W:(c % 2 + 1) * W], YA[0:nj, e, jt, c * 128:(c + 1) * 128], s_[0:nj, jt, :], False, lastmm,
                                        ['YA', ks_], [kps], skip_group_check=True)
                    for c in range(KC):
                        ps, kps = acc[c // 2]
                        self.STT(xb[:, c, :], ps[:, (c % 2) * W:(c % 2 + 1) * W], self.modv[:, 40 + c, j:j + 1], xb[:, c, :], ALU.mult, ALU.add,
                                 [kps, 'modv', kx], [kx])
                    self.DMA('sp', self.xT[:, :, t0:t0 + W], xb[:], [kx], [('xTb', bi)])

    def phase_final(self):
        with self.scope() as S:
            xbs = [S.sb("fn_xb%d" % i, [128, KC, 512], F32) for i in range(2)]
            sqs = [S.sb("fn_sq%d" % i, [128, 512], BF16) for i in range(2)]
            rss = [S.sb("fn_rs%d" % i, [128, 512], F32) for i in range(2)]
            yb = S.sb("fn_y", [128, KC, 512], F32)
            ot = [S.sb("fn_o%d" % i, [128, 1024], F32) for i in range(2)]
            cnt = 0
            for bi, (t0, W) in enumerate(BLOCKS):
                if bi == 0:
                    continue
                xb = xbs[bi % 2]; kx = 'xb%d' % (bi % 2)
                self.DMA('sp', xb[:, :, :W], self.xT[:, :, t0:t0 + W], [], [kx])
                rs, krs = self.norm_block(S, xb, kx, W, bi, sqs, rss)
                for c in range(KC):
                    self.STT(yb[:, c, :W], xb[:, c, :W], self.fngs[:, c:c + 1], rs[:, :W], ALU.mult, ALU.mult, [kx, krs, 'fngs'], [('y', c)])
                for jt in range(W // 128):
                    o = ot[cnt % 2]; ko = 'fo%d' % (cnt % 2); cnt += 1
                    for hf in range(2):
                        ps, kps = ((self.P0, 'P0'), (self.P1, 'P1'))[hf]
                        for c4 in range(4):
                            c = hf * 4 + c4
                            self.TR(ps[:, c4 * 128:(c4 + 1) * 128], yb[:, c, jt * 128:(jt + 1) * 128], self.ident[:], [('y', c), 'ident'], [kps])
                        self.CP('act' if hf else 'dve', o[:, hf * 512:(hf + 1) * 512], ps[:, :], [kps], [ko])
                    r0 = t0 - TC + jt * 128
                    self.DMA('sp', self.out[r0:r0 + 128, :], o[:], [ko], [('out', r0)])

    def debug_dump(self):
        if getattr(self, '_dumped', False):
            return
        self._dumped = True
        with self.scope() as S:
            for name in self.dbg:
                if name == 'hT':
                    o = self.dout("dbg_hT", [128, KC, T], BF16)
                    self.DMA('sp', o, self.hT[:], [], ['dbg_hT'])
                elif name == 'modv':
                    o = self.dout("dbg_modv", [128, 96], F32)
                    self.DMA('sp', o, self.modv[:].rearrange("p m j -> p (m j)"), [], ['dbg_modv'])
                elif name in ('xT', 'oa', 'ob', 'oc', 'ga'):
                    dt = F32 if name == 'xT' else BF16
                    srcd = getattr(self, name)
                    o = self.dout("dbg_" + name, [128, KC, T], dt)
                    xb = S.sb("dbg_xb_" + name, [128, KC, 512], dt)
                    for bi, (t0, W) in enumerate(BLOCKS):
                        self.DMA('sp', xb[:, :, :W], srcd[:, :, t0:t0 + W], [], ['dxb'])
                        self.DMA('sp', o[:, :, t0:t0 + W], xb[:, :, :W], ['dxb'], ['dxo'])

def _consts():
    ident = np.eye(128, dtype=np.float32)
    blk64 = np.zeros((128, 128), np.float32); blk64[:64, :64] = 1; blk64[64:, 64:] = 1
    swap = np.zeros((128, 128), np.float32)
    for j in range(64):
        swap[2 * j + 1, 2 * j] = 1; swap[2 * j, 2 * j + 1] = 1
    s = np.arange(32)
    maskF = np.tile((s[:, None] <= s[None, :]).astype(np.float32), (4, 1))
    maskB = np.tile((s[:, None] >= s[None, :]).astype(np.float32), (4, 1))
    rowm = np.zeros((128, 4), np.float32)
    for q_ in range(4):
        rowm[q_ * 32:(q_ + 1) * 32, q_] = 1.0
    mask01 = np.ones((128, 512), np.float32); mask01[:, 0::32] = 0
    iota = np.tile(np.arange(512, dtype=np.float32)[None, :], (128, 1))
    jcol = (np.arange(128, dtype=np.float32)[:, None] + 128 * np.arange(4, dtype=np.float32)[None, :]).astype(np.float32)
    rows = TL // 64
    row = np.repeat(np.arange(rows), 64).astype(np.float32); col = np.tile(np.arange(64), rows).astype(np.float32)
    inv = (np.float32(10000.0) ** (-np.arange(0, 32, 2, dtype=np.float32) / np.float32(32))).astype(np.float32)
    ang = np.concatenate([row[:, None] * inv, col[:, None] * inv], axis=-1).astype(np.float32)
    cos = np.cos(ang).astype(np.float32); sin = np.sin(ang).astype(np.float32)
    p = np.arange(128); pj = (p % 64) // 2
    cosT = np.ascontiguousarray(cos[:, pj].T)
    sgn = np.where(p % 2 == 0, -1.0, 1.0).astype(np.float32)
    sinS = np.ascontiguousarray((sin[:, pj] * sgn[None, :]).T)
    return dict(c_ident=ident, c_blk64=blk64, c_swap=swap, c_maskF=maskF, c_maskB=maskB, c_rowm=rowm, c_mask01=mask01, c_iota=iota, c_jcol=jcol,
                c_cos=cosT, c_sin=sinS)


def _cols(v):
    return np.ascontiguousarray(np.swapaxes(v.reshape(v.shape[:-1] + (8, 128)), -1, -2))


def prep_inputs(inp):
    shared = dict(_consts())
    f = lambda a: np.ascontiguousarray(np.asarray(a, dtype=np.float32))
    shared['ada_w'] = f(inp['ada_w'])
    shared['ada_b'] = np.ascontiguousarray(np.swapaxes(f(inp['ada_b']).reshape(DEPTH, 48, 128), 1, 2))
    shared['n1g'] = _cols(f(inp['norm1_g'])); shared['n2g'] = _cols(f(inp['norm2_g'])); shared['fng'] = _cols(f(inp['final_norm_g']))
    shared['hgn'] = _cols(f(inp['hg_norm_g']))
    cw = f(inp['conv_w'])
    shared['convw'] = np.ascontiguousarray(np.concatenate([_cols(cw[:, k]) for k in range(3)], axis=-1))
    shared['qng'] = np.ascontiguousarray(np.tile(f(inp['q_norm_g']), (1, 2))[:, :, None])
    shared['kng'] = np.ascontiguousarray(np.tile(f(inp['k_norm_g']), (1, 2))[:, :, None])
    lbl = f(inp['hg_lb_logits'])
    shared['lbl'] = np.ascontiguousarray(_cols(lbl).transpose(2, 0, 1, 3).reshape(128, 32))
    w_in = f(inp['w_in'])
    shared['w_in'] = w_in
    wk = w_in[:, :, O_AK:O_AK + 256]
    shared['wk_dup'] = np.ascontiguousarray(np.concatenate([np.concatenate([wk[:, :, g * 64:(g + 1) * 64]] * 2, axis=-1) for g in range(4)], axis=-1))
    for k in ('w_proj_a', 'w_proj_b', 'w_proj_c', 'w_out', 'router_w', 'w_gate', 'w_up', 'w_down'):
        shared[k] = f(inp[k])
    x = f(inp['x']); ctx = f(inp['ctx']); c = f(inp['c']); c_ctx = f(inp['c_ctx'])
    maps = []
    for b in range(x.shape[0]):
        m = dict(shared)
        m['x'] = x[b]; m['ctx'] = ctx[b]
        m['cc'] = np.ascontiguousarray(np.concatenate([c[b].reshape(8, 128).T, c_ctx.reshape(8, 128).T], axis=1))
        maps.append(m)
    return maps


def kernel(**inputs):
    maps = prep_inputs(inputs)
    prog = Prog()
    nc = prog.build()
    maps = [{k: v for k, v in m.items() if k in prog.inputs} for m in maps]
    res = run_bass_kernel_spmd(nc, maps, core_ids=list(range(8)))
    return np.stack([np.asarray(r["out"], dtype=np.float32) for r in res.results], axis=0)
```
